# Optimizing a Trainium2 kernel written in Bass

```python
import jax, jax.numpy as jnp
from jax import lax
import numpy as np

D_MODEL = 1024
BATCH = 32
SEQ = 2048
DEPTH = 2

HEAD_DIM = 64
N_SB_HEADS = 8
D_SB = N_SB_HEADS * HEAD_DIM
N_SGU_GROUPS = 8
D_SGU = N_SGU_GROUPS * HEAD_DIM
N_GROUPS = N_SB_HEADS + N_SGU_GROUPS
D_MIX = D_SB + D_SGU
D_IN = 3 * D_SB + 2 * D_SGU
BLOCK = 128
CHUNK = 128
D_FF = 2816
N_EXPERTS = 8
TOP_K = 2
D_FF_EXPERT = 1408
N_DENSE = (DEPTH + 1) // 2
N_MOE = DEPTH // 2
EPS = 1e-6

kernel_name = "hybrid_stickbreak_sgu_moe_block"


def rms_norm(x, g):
    xf = x.astype(jnp.float32)
    var = jnp.mean(xf * xf, axis=-1, keepdims=True)
    return (xf * lax.rsqrt(var + EPS)).astype(x.dtype) * g


def group_rms_norm(x, g):
    return rms_norm(x, g.reshape(x.shape[-2], x.shape[-1]))


def stick_breaking_attention(q, k, v):
    seq = q.shape[1]
    scale = HEAD_DIM ** -0.5
    outs = []
    for i in range(seq // BLOCK):
        q0 = i * BLOCK
        end = q0 + BLOCK
        qb = q[:, q0:end]
        kb = k[:, :end]
        vb = v[:, :end]
        z = jnp.einsum('bqhd,bkhd->bhqk', qb.astype(jnp.float32), kb.astype(jnp.float32)) * scale
        qpos = q0 + jnp.arange(BLOCK)[:, None]
        kpos = jnp.arange(end)[None, :]
        causal = kpos < qpos
        log_one_minus = jnp.where(causal, -jax.nn.softplus(z), 0.0)
        between = lax.cumsum(log_one_minus, axis=3, reverse=True) - log_one_minus
        a = jnp.where(causal, jnp.exp(jax.nn.log_sigmoid(z) + between), 0.0)
        outs.append(jnp.einsum('bhqk,bkhd->bqhd', a.astype(v.dtype), vb))
    return jnp.concatenate(outs, axis=1)


def chunked_spatial_gating(u, gate, w_s, b_s, g_gate):
    bsz, seq, _ = u.shape
    u = jax.nn.gelu(u).reshape(bsz, seq, N_SGU_GROUPS, HEAD_DIM)
    gate = jax.nn.gelu(gate).reshape(bsz, seq, N_SGU_GROUPS, HEAD_DIM)
    gate = group_rms_norm(gate, g_gate)
    gate = gate.reshape(bsz, seq // CHUNK, CHUNK, N_SGU_GROUPS, HEAD_DIM)
    w_causal = jnp.tril(w_s)
    mixed = jnp.einsum('gts,bcsgd->bctgd', w_causal, gate) + b_s.T[None, None, :, :, None]
    return u * mixed.reshape(bsz, seq, N_SGU_GROUPS, HEAD_DIM)


def swiglu(x, wg, wu, wd):
    return (jax.nn.silu(x @ wg) * (x @ wu)) @ wd


def moe_swiglu(h, router_w, wg, wu, wd):
    bsz, seq, d = h.shape
    xt = h.reshape(-1, d)
    logits = (xt @ router_w).astype(jnp.float32)
    top_logits, top_idx = lax.top_k(logits, TOP_K)
    top_w = jax.nn.softmax(top_logits, axis=-1)
    gates = jnp.sum(jax.nn.one_hot(top_idx, N_EXPERTS, dtype=jnp.float32) * top_w[..., None], axis=1)
    gates = gates.astype(h.dtype)
    out = jnp.zeros_like(xt)
    for e in range(N_EXPERTS):
        out = out + gates[:, e:e + 1] * swiglu(xt, wg[e], wu[e], wd[e])
    return out.reshape(bsz, seq, d)


def setup_inputs(seed: int = 0) -> dict:
    key = jax.random.key(seed)
    ks = jax.random.split(key, 17)
    f32 = jnp.float32
    nrm = lambda k, shape, s: jax.random.normal(k, shape, f32) * s
    gain = lambda k, shape: 1.0 + 0.05 * jax.random.normal(k, shape, f32)
    return {
        "x": jax.random.normal(ks[0], (BATCH, SEQ, D_MODEL), f32),
        "w_in": nrm(ks[1], (DEPTH, D_MODEL, D_IN), D_MODEL ** -0.5),
        "w_out": nrm(ks[2], (DEPTH, D_MIX, D_MODEL), D_MIX ** -0.5),
        "g_mix": gain(ks[3], (DEPTH, D_MODEL)),
        "g_ffn": gain(ks[4], (DEPTH, D_MODEL)),
        "g_sgu": gain(ks[5], (DEPTH, D_SGU)),
        "sgu_w": nrm(ks[6], (DEPTH, N_SGU_GROUPS, CHUNK, CHUNK), CHUNK ** -0.5),
        "sgu_b": 1.0 + 0.1 * jax.random.normal(ks[7], (DEPTH, N_SGU_GROUPS, CHUNK), f32),
        "g_out": gain(ks[8], (DEPTH, D_MIX)),
        "ffn_w_gate": nrm(ks[9], (N_DENSE, D_MODEL, D_FF), D_MODEL ** -0.5),
        "ffn_w_up": nrm(ks[10], (N_DENSE, D_MODEL, D_FF), D_MODEL ** -0.5),
        "ffn_w_down": nrm(ks[11], (N_DENSE, D_FF, D_MODEL), D_FF ** -0.5),
        "router_w": nrm(ks[12], (N_MOE, D_MODEL, N_EXPERTS), D_MODEL ** -0.5),
        "moe_w_gate": nrm(ks[13], (N_MOE, N_EXPERTS, D_MODEL, D_FF_EXPERT), D_MODEL ** -0.5),
        "moe_w_up": nrm(ks[14], (N_MOE, N_EXPERTS, D_MODEL, D_FF_EXPERT), D_MODEL ** -0.5),
        "moe_w_down": nrm(ks[15], (N_MOE, N_EXPERTS, D_FF_EXPERT, D_MODEL), D_FF_EXPERT ** -0.5),
        "g_final": gain(ks[16], (D_MODEL,)),
    }


def reference(x, w_in, w_out, g_mix, g_ffn, g_sgu, sgu_w, sgu_b, g_out,
              ffn_w_gate, ffn_w_up, ffn_w_down, router_w,
              moe_w_gate, moe_w_up, moe_w_down, g_final):
    bsz, seq, _ = x.shape
    for l in range(DEPTH):
        h = rms_norm(x, g_mix[l])
        proj = h @ w_in[l]
        q, k, v, u, gate = jnp.split(
            proj, [D_SB, 2 * D_SB, 3 * D_SB, 3 * D_SB + D_SGU], axis=-1)
        q = q.reshape(bsz, seq, N_SB_HEADS, HEAD_DIM)
        k = k.reshape(bsz, seq, N_SB_HEADS, HEAD_DIM)
        v = v.reshape(bsz, seq, N_SB_HEADS, HEAD_DIM)
        y_sb = stick_breaking_attention(q, k, v)
        y_sgu = chunked_spatial_gating(u, gate, sgu_w[l], sgu_b[l], g_sgu[l])
        y = jnp.concatenate([y_sb, y_sgu], axis=2)
        y = group_rms_norm(y, g_out[l]).reshape(bsz, seq, D_MIX)
        x = x + y @ w_out[l]
        h2 = rms_norm(x, g_ffn[l])
        if l % 2 == 0:
            i = l // 2
            x = x + swiglu(h2, ffn_w_gate[i], ffn_w_up[i], ffn_w_down[i])
        else:
            i = l // 2
            x = x + moe_swiglu(h2, router_w[i], moe_w_gate[i], moe_w_up[i], moe_w_down[i])
    return rms_norm(x, g_final)
```

```python
import contextlib
from collections import deque

import numpy as np
import concourse.bass as bass
import concourse.mybir as mybir
from concourse.bass_utils import run_bass_kernel_spmd

F32 = mybir.dt.float32
BF16 = mybir.dt.bfloat16
AF = mybir.ActivationFunctionType
ALU = mybir.AluOpType
AX = mybir.AxisListType

D = 1024
SEQ = 2048
NT = SEQ // 128
DEPTH = 2
D_IN = 2560
D_FF = 2816
NE = 8
D_FFE = 1408
EPS = 1e-6
NCORES = 8
SEQ_PER_CORE = 4
NSLOT = 8


def I(name, *a, **kw):
    return lambda e: getattr(e, name)(*a, **kw)


class R:
    __slots__ = ("w", "r")

    def __init__(self):
        self.w = None
        self.r = {}


class Sched:
    ENGS = ("pe", "act", "dve", "pool", "sp")

    def __init__(self, nc, stack, n_dma_sems=32, n_fresh=0):
        self.nc = nc
        self.dry = False
        self.fresh = [stack.enter_context(nc.semaphore("s_fr%d" % i)) for i in range(n_fresh)]
        self.fresh_i = 0
        self.q = {e: [] for e in self.ENGS}
        self.sems = {}
        self.count = {}
        for e in self.ENGS:
            self.sems[e] = stack.enter_context(nc.semaphore("s_" + e))
            self.count[e] = 0
        self.n_dma = n_dma_sems
        for i in range(n_dma_sems):
            k = ("dma", i)
            self.sems[k] = stack.enter_context(nc.semaphore("s_dma%d" % i))
            self.count[k] = 0
        self.dma_rr = 0
        self.n_pdma = 16
        for i in range(self.n_pdma):
            k = ("pdma", i)
            self.sems[k] = stack.enter_context(nc.semaphore("s_pdma%d" % i))
            self.count[k] = 0
        self.pdma_rr = 0
        self.seen = {e: {} for e in self.ENGS}
        self.n_wait = 0
        self.n_ins = 0

    def _waits(self, eng, reads, writes, extra=()):
        need = {}

        def add(st):
            if st is None:
                return
            k, v = st
            if need.get(k, 0) < v:
                need[k] = v

        for r in reads:
            add(r.w)
        for w in writes:
            add(w.w)
            for k, v in w.r.items():
                add((k, v))
        for st in extra:
            add(st)
        out = []
        seen = self.seen[eng]
        for k, v in need.items():
            if seen.get(k, 0) >= v:
                continue
            seen[k] = v
            out.append((self.sems[k], v))
        return out

    def _mark(self, stamp, reads, writes):
        k, v = stamp
        for w in writes:
            w.w = stamp
            w.r = {}
        for r in reads:
            if r.r.get(k, 0) < v:
                r.r[k] = v

    def op(self, eng, fns, reads=(), writes=()):
        if self.dry:
            return None
        if callable(fns):
            fns = [fns]
        waits = self._waits(eng, reads, writes)
        self.count[eng] += 1
        stamp = (eng, self.count[eng])
        sem = self.sems[eng]
        self.n_wait += len(waits)
        self.n_ins += len(fns)

        def thunk(e, waits=waits, fns=fns, sem=sem):
            for s, v in waits:
                e.wait_ge(s, v)
            last = None
            for f in fns:
                last = f(e)
            last.then_inc(sem, 1)

        self.q[eng].append(thunk)
        self._mark(stamp, reads, writes)
        return stamp

    def dma(self, eng, fn, reads=(), writes=()):
        if self.dry:
            return None
        if eng == "pool" and self.fresh:
            k = ("fresh", self.fresh_i)
            self.sems[k] = self.fresh[self.fresh_i]
            self.count[k] = 0
            self.fresh_i += 1
        elif eng == "pool":
            i = self.pdma_rr
            self.pdma_rr = (self.pdma_rr + 1) % self.n_pdma
            k = ("pdma", i)
        else:
            i = self.dma_rr
            self.dma_rr = (self.dma_rr + 1) % self.n_dma
            k = ("dma", i)
        prev = (k, self.count[k]) if self.count[k] else None
        waits = self._waits(eng, reads, writes, extra=(prev,) if prev else ())
        self.count[k] += 16
        stamp = (k, self.count[k])
        sem = self.sems[k]
        self.n_wait += len(waits)
        self.n_ins += 1

        def thunk(e, waits=waits, fn=fn, sem=sem):
            for s, v in waits:
                e.wait_ge(s, v)
            fn(e).then_inc(sem, 16)

        self.q[eng].append(thunk)
        self._mark(stamp, reads, writes)
        return stamp

    def final_wait(self, eng, stamps):
        need = {}
        for k, v in stamps:
            if need.get(k, 0) < v:
                need[k] = v
        ws = [(self.sems[k], v) for k, v in need.items()]

        def thunk(e, ws=ws):
            for s, v in ws:
                e.wait_ge(s, v)

        self.q[eng].append(thunk)

    def emit(self, block):
        q = self.q

        @block.tensor
        def _(e):
            for t in q["pe"]:
                t(e)

        @block.scalar
        def _(e):
            for t in q["act"]:
                t(e)

        @block.vector
        def _(e):
            for t in q["dve"]:
                t(e)

        @block.gpsimd
        def _(e):
            for t in q["pool"]:
                t(e)

        @block.sync
        def _(e):
            for t in q["sp"]:
                t(e)


class Lanes:
    def __init__(self, stages, est_ticks):
        self.stages = [list(st) for st in stages if st]
        self.est = max(1, est_ticks)
        self.used = 0

    def done(self):
        return not self.stages

    def tick(self, k=1):
        for _ in range(k):
            if not self.stages:
                return
            self.used += 1
            lanes = self.stages[0]
            alive = []
            for g in lanes:
                if next(g, "END") != "END":
                    alive.append(g)
            if alive:
                self.stages[0] = alive
            else:
                self.stages.pop(0)

    def ticks_needed(self, steps_left):
        rem = max(1, self.est - self.used)
        return max(1, (rem + max(1, steps_left) - 1) // max(1, steps_left))


class WStream:
    def __init__(self, S, slots, nslot):
        self.S = S
        self.slots = slots
        self.R = [R() for _ in range(nslot)]
        self.nslot = nslot
        self.plan = []
        self.record = True
        self.i = 0
        self.issued = 0
        self.free = list(range(nslot))
        self.slot_of = {}

    def start_real(self):
        self.record = False
        self.i = 0
        self.issued = 0
        self.free = list(range(self.nslot))
        self.slot_of = {}

    def _view(self, k, shape):
        n = 1
        for s_ in shape[1:]:
            n *= s_
        v = self.slots[k][:, 0:n]
        if len(shape) == 3:
            v = v.rearrange("p (a b) -> p a b", b=shape[2])
        return v

    def _prefetch(self):
        while self.issued < len(self.plan) and self.free and self.issued - self.i < 4:
            j = self.issued
            k = self.free.pop(0)
            src, shape = self.plan[j]
            dst = self._view(k, shape)
            self.S.dma("pool", lambda e, dst=dst, src=src: e.dma_start(out=dst, in_=src), writes=[self.R[k]])
            self.slot_of[j] = k
            self.issued += 1

    def next(self, src, shape):
        i = self.i
        self.i += 1
        if self.record:
            self.plan.append((src, shape))
            return self._view(0, shape), self.R[0], None
        self._prefetch()
        assert i in self.slot_of, "weight ring exhausted: too many live slices"
        k = self.slot_of[i]
        return self._view(k, shape), self.R[k], (i, k)

    def release(self, handle):
        if handle is None:
            return
        i, k = handle
        self.free.append(k)
        self._prefetch()


def build(nseq=SEQ_PER_CORE, depth=DEPTH, stop=None, n_fresh=0, ndummy=0):
    nc = bass.Bass("TRN2", target_bir_lowering=False)
    dt = lambda name, shape: nc.dram_tensor(name, shape, F32, kind="ExternalInput").ap()
    x_d = dt("x", [nseq, SEQ, D])
    w_in_d = dt("w_in", [DEPTH, D, D_IN])
    w_out_d = dt("w_out", [DEPTH, D, D])
    g_mix_d = dt("g_mix", [DEPTH, D])
    g_ffn_d = dt("g_ffn", [DEPTH, D])
    g_sgu_d = dt("g_sgu", [DEPTH, 512])
    sgu_w_d = dt("sgu_w", [DEPTH, 8, 128, 128])
    sgu_b_d = dt("sgu_b", [DEPTH, 8, 128])
    g_out_d = dt("g_out", [DEPTH, D])
    ffn_wg_d = dt("ffn_w_gate", [1, D, D_FF])
    ffn_wu_d = dt("ffn_w_up", [1, D, D_FF])
    ffn_wd_d = dt("ffn_w_down", [1, D_FF, D])
    router_d = dt("router_w", [1, D, NE])
    moe_wg_d = dt("moe_w_gate", [1, NE, D, D_FFE])
    moe_wu_d = dt("moe_w_up", [1, NE, D, D_FFE])
    moe_wd_d = dt("moe_w_down", [1, NE, D_FFE, D])
    g_final_d = dt("g_final", [D])
    out_d = nc.dram_tensor("out", [nseq, SEQ, D], F32, kind="ExternalOutput").ap()

    with contextlib.ExitStack() as st:
        T = lambda name, shape, dtype=F32: st.enter_context(nc.sbuf_tensor(name, shape, dtype))
        x_sb = T("x_sb", [128, NT, D])
        big = T("big", [128, 16384], BF16)
        kT = big[:, 0:8192].rearrange("p (c t) -> p c t", t=SEQ)
        v_sb = big[:, 8192:16384].rearrange("p (t d) -> p t d", d=512)
        h2T = big[:, :].rearrange("p (k t) -> p k t", t=SEQ)
        Rbig = [R() for _ in range(32)]
        hT = T("hT", [128, 8, 512], BF16)
        qT2 = [T("qT%d" % i, [128, 4, 512], BF16) for i in range(2)]
        qT = qT2[0]
        uT = T("uT", [128, 4, 512], BF16)
        gate_n = T("gate_n", [128, 4, 512], BF16)
        y_sb = T("y_sb", [128, 8, 512], BF16)
        slots = [T("wslot%d" % i, [128, 2048], BF16) for i in range(NSLOT)]
        E_sb = [T("E%d" % i, [128, 1024], BF16) for i in range(3)]
        P_sb = [T("P%d" % i, [128, 1024], BF16) for i in range(2)]
        G_sb = [T("G%d" % i, [128, 1024], BF16) for i in range(2)]
        A_sb = [T("A0", [128, 1024], BF16)]
        PS_sb = T("PS", [128, 1024], BF16)
        xn0 = T("xn0", [128, D], BF16)
        xn = [xn0, xn0]
        junk = T("junk", [128, D], BF16)
        ss = T("ss", [128, NT])
        lnv = T("lnv", [128, NT])
        rstd = T("rstd", [128, NT])
        gss = T("gss", [128, 32])
        glv = T("glv", [128, 32])
        grs = T("grs", [128, 32])
        sqb = [T("sqb%d" % i, [128, 512], BF16) for i in range(2)]
        sg_sb = [T("sg%d" % i, [128, 512]) for i in range(2)]
        aT = [qT[:, 0:2, :], uT[:, 0:2, :]]
        etmp = [T("etmp%d" % i, [128, 512]) for i in range(2)]
        stmp = etmp[0]
        nl = [etmp[1], sg_sb[1]]
        ident = T("ident", [128, 128], BF16)
        tri = T("tri", [128, 128], BF16)
        ones = T("ones", [128, 128], BF16)
        bones = T("bones", [128, 128], BF16)
        mask = T("mask", [128, 128], BF16)
        cf = T("cf", [128, 128])
        gain_bc = T("gain_bc", [128, D])
        gsgu_bc = T("gsgu_bc", [128, 512])
        B_sgu = T("B_sgu", [128, 4, 128])
        gout_c = T("gout_c", [128, 8])
        WsT = T("WsT", [128, DEPTH, 8, 128], BF16)
        wtmp = gain_bc[:, :].rearrange("p (g t) -> p g t", t=128)
        wtmpb = junk[:, :].rearrange("p (g t) -> p g t", t=128)
        router_sb = T("router_sb", [128, 8, NE], BF16)
        logit = T("logit", [128, NE])
        m1 = T("m1", [128, 4])
        mk1 = T("mk1", [128, NE])
        mk2 = T("mk2", [128, NE])
        l2 = T("l2", [128, NE])
        gates = T("gates", [128, NT, NE])

        banks = []
        for i in range(3):
            if i == 2:
                banks.append(st.enter_context(nc.psum_tensor("bank2", [128, 1024], BF16)))
            else:
                banks.append(st.enter_context(nc.psum_tensor("bank%d" % i, [128, 512], F32)))
        zz = st.enter_context(nc.psum_tensor("zz", [128, 1024], F32))
        cc = st.enter_context(nc.psum_tensor("cc", [128, 1024], F32))
        banks += [zz[:, 0:512], zz[:, 512:1024], cc[:, 0:512], cc[:, 512:1024]]
        banks.append(st.enter_context(nc.psum_tensor("bank7", [128, 512], F32)))
        pst = banks[2]
        bank2f = banks[2][:, :].bitcast(F32)
        Rbank = [R() for _ in range(8)]
        Rpst = [Rbank[2], Rbank[2]]
        S = Sched(nc, st, n_dma_sems=(8 if n_fresh else 24), n_fresh=n_fresh)
        ws = WStream(S, slots, NSLOT)

        Rx = [R() for _ in range(NT)]
        RhT = R(); RqT2 = [R(), R()]; RqT = RqT2[0]; RuT = R(); Rgn = [R() for _ in range(4)]
        Ry = [R() for _ in range(8)]
        RE = [R(), R(), R()]; RP = [R(), R()]; RG = [R(), R()]; RA = [R()]; RPS = [R(), R()]
        Rxn0 = R(); Rxn = [Rxn0, Rxn0]; Rjunk = R(); Rss = [R() for _ in range(NT)]
        Rgst = [R() for _ in range(4)]
        Rsqb = [R(), R()]; Rsg = [R(), R()]; RaT = [RqT, RuT]; Retmp = [R(), R()]; Rstmp = Retmp[0]; Rnl = [Retmp[1], Rsg[1]]
        Rconst = R(); Rgain = R(); Rgsgu = R(); RB = R(); Rgout = R(); RWs = R(); Rwtmp = Rgain; Rwtmpb = Rjunk; Rcf = R()
        Rrouter = R(); Rlog = R(); Rgates = [R() for _ in range(NT)]

        acc_rr = [0]

        def acc_bank():
            b = acc_rr[0]
            acc_rr[0] = (b + 1) % 2
            return b

        cnt = {"z": 0, "c": 0, "u": 0, "ev": 0, "et": 0}

        def setup_consts():
            def build_mask(dst, pattern, cmp_op, cm):
                S.op("pool", I("memset", cf[:], 1.0), writes=[Rcf])
                S.op("pool", I("affine_select", out=cf[:], in_=cf[:], pattern=pattern, compare_op=cmp_op,
                               fill=0.0, base=0, channel_multiplier=cm), reads=[Rcf], writes=[Rcf])
                S.op("dve", I("tensor_copy", out=dst[:], in_=cf[:]), reads=[Rcf], writes=[Rconst])
            build_mask(ident, [[-1, 128]], ALU.is_equal, 1)
            build_mask(tri, [[-1, 128]], ALU.is_ge, 1)
            S.op("pool", I("memset", cf[:], -30000.0), writes=[Rcf])
            S.op("pool", I("affine_select", out=cf[:], in_=cf[:], pattern=[[-1, 128]], compare_op=ALU.is_ge,
                           fill=0.0, base=0, channel_multiplier=1), reads=[Rcf], writes=[Rcf])
            S.op("dve", I("tensor_copy", out=mask[:], in_=cf[:]), reads=[Rcf], writes=[Rconst])
            S.op("dve", [I("memset", ones[:], 1.0), I("memset", bones[:], 0.0)], writes=[Rconst])
            S.op("dve", [I("memset", bones[0:64, 0:64], 1.0), I("memset", bones[64:128, 64:128], 1.0)], writes=[Rconst])
            for l in range(depth):
                S.dma("sp", I("dma_start", out=wtmp[:], in_=sgu_w_d[l].rearrange("g t s -> t g s")), writes=[Rwtmp])
                S.op("dve", I("tensor_copy", out=wtmpb[:], in_=wtmp[:]), reads=[Rwtmp], writes=[Rwtmpb])
                S.op("pe", [I("transpose", out=pst[:, g * 128:(g + 1) * 128], in_=wtmpb[:, g, :], identity=ident[:])
                            for g in range(8)], reads=[Rwtmpb, Rconst], writes=[Rbank[2]])
                S.op("dve", I("tensor_copy", out=wtmpb[:].rearrange("p g t -> p (g t)"), in_=pst[:, 0:1024]),
                     reads=[Rbank[2]], writes=[Rwtmpb])
                S.op("pool", [I("affine_select", out=WsT[:, l, g, :], in_=wtmpb[:, g, :], pattern=[[1, 128]],
                                compare_op=ALU.is_ge, fill=0.0, base=0, channel_multiplier=-1) for g in range(8)],
                     reads=[Rwtmpb], writes=[RWs])
            if depth > 1:
                S.dma("pool", I("dma_start", out=router_sb[:], in_=router_d[0].rearrange("(k p) e -> p k e", p=128)),
                      writes=[Rrouter])

        def rstd_batch_gen(tiles):
            t0, t1 = tiles[0], tiles[-1] + 1
            Rs = [Rss[t] for t in tiles]
            S.op("pool", I("memset", ss[:, t0:t1], 0.0), writes=Rs)
            yield
            for t in tiles:
                S.op("act", I("activation", out=junk[:], in_=x_sb[:, t, :], func=AF.Square, accum_out=ss[:, t:t + 1]),
                     reads=[Rx[t]], writes=[Rjunk, Rss[t]])
                yield
            S.op("act", I("activation", out=lnv[:, t0:t1], in_=ss[:, t0:t1], func=AF.Ln, bias=EPS, scale=1.0 / D), reads=Rs, writes=Rs)
            yield
            S.op("act", I("activation", out=rstd[:, t0:t1], in_=lnv[:, t0:t1], func=AF.Exp, scale=-0.5), reads=Rs, writes=Rs)
            yield

        def norm_gen(t, dest, Rdest, col0, i=None):
            if i is None:
                i = cnt["u"] % 2
                cnt["u"] += 1
            S.op("dve", I("scalar_tensor_tensor", out=xn[i][:], in0=x_sb[:, t, :], scalar=rstd[:, t:t + 1], in1=gain_bc[:],
                          op0=ALU.mult, op1=ALU.mult), reads=[Rx[t], Rss[t], Rgain], writes=[Rxn[i]])
            yield
            ph = pst[:, i * 512:(i + 1) * 512]
            for half in range(2):
                S.op("pe", [I("transpose", out=ph[:, k * 128:(k + 1) * 128], in_=xn[i][:, (half * 4 + k) * 128:(half * 4 + k + 1) * 128],
                              identity=ident[:]) for k in range(4)], reads=[Rxn[i], Rconst], writes=[Rpst[i]])
                yield
                src = ph.rearrange("p (k t) -> p k t", t=128)
                dst = dest[:, half * 4:(half + 1) * 4, col0:col0 + 128]
                if cnt["ev"] % 2 == 0:
                    S.op("dve", I("tensor_copy", out=dst, in_=src), reads=[Rpst[i]], writes=Rdest)
                else:
                    S.op("act", I("activation", out=dst, in_=src, func=AF.Copy), reads=[Rpst[i]], writes=Rdest)
                cnt["ev"] += 1
                yield

        def norm_tile(t, dest, Rdest, col0):
            for _ in norm_gen(t, dest, Rdest, col0):
                pass

        def load_gain(g_ap):
            S.dma("sp", I("dma_start", out=gain_bc[:], in_=g_ap.partition_broadcast(128)), writes=[Rgain])

        def attention_block(blk, side=None, first=None):
            bo = 7
            qT = qT2[blk % 2]
            RqT = RqT2[blk % 2]
            Rzz = [Rbank[3], Rbank[4]]
            Rcc = [Rbank[5], Rbank[6]]
            units = [(c, kb) for c in range(4) for kb in range(4 * blk + 3, -1, -1)]
            n = len(units)
            V3 = lambda ap: ap.rearrange("p (h q) -> p h q", h=2)

            def prm(i):
                c, kb = units[i]
                col0 = max(0, kb * 128 - blk * 512)
                return c, kb, col0, kb >= 4 * blk, kb == 4 * blk + 3

            def stA1(i):
                c, kb, col0, diag, first_ = prm(i)
                E = E_sb[i % 3]
                fz = []
                for hi in range(2):
                    hb = hi * 64
                    fz.append(I("matmul", zz[:, hi * 512 + col0:hi * 512 + 512], kT[hb:hb + 64, c, kb * 128:(kb + 1) * 128],
                                qT[hb:hb + 64, c, col0:512], start=True, stop=not diag, skip_group_check=True))
                if diag:
                    for hi in range(2):
                        fz.append(I("matmul", zz[:, hi * 512 + col0:hi * 512 + col0 + 128], ident[:], mask[:], start=False, stop=True,
                                    skip_group_check=True))
                S.op("pe", fz, reads=[Rbig[c * 4 + kb // 4], RqT, Rconst], writes=Rzz)
                S.op("act", I("activation", out=V3(E[:, :])[:, :, col0:512], in_=V3(zz[:, :])[:, :, col0:512], func=AF.Exp),
                     reads=Rzz, writes=[RE[i % 3]])

            def stA2(i):
                c, kb, col0, diag, first_ = prm(i)
                E, P = E_sb[i % 3], P_sb[i % 2]
                S.op("act", I("activation", out=V3(P[:, :])[:, :, col0:512], in_=V3(E[:, :])[:, :, col0:512], func=AF.Ln, bias=1.0),
                     reads=[RE[i % 3]], writes=[RP[i % 2]])

            def stB(i):
                c, kb, col0, diag, first_ = prm(i)
                P, G, PS = P_sb[i % 2], G_sb[i % 2], PS_sb
                for hi in range(2):
                    o = hi * 512
                    fns = [I("matmul", cc[:, o + col0:o + 512], tri[:], P[:, o + col0:o + 512], start=True, stop=first_,
                             skip_group_check=True)]
                    if not first_:
                        fns.append(I("matmul", cc[:, o + col0:o + 512], ones[:], PS[:, o + col0:o + 512], start=False, stop=True,
                                     skip_group_check=True))
                    S.op("pe", fns, reads=[RP[i % 2], Rconst] + ([] if first_ else [RPS[hi]]), writes=[Rcc[hi]])
                    if kb > 0:
                        if first_:
                            fl = [I("tensor_copy", out=PS[:, o + col0:o + 512], in_=P[:, o + col0:o + 512])]
                            if col0 > 0:
                                fl.append(I("memset", PS[:, o:o + col0], 0.0))
                            S.op("pool", fl, reads=[RP[i % 2]], writes=[RPS[hi]])
                        else:
                            S.op("pool", I("tensor_tensor", out=PS[:, o + col0:o + 512], in0=PS[:, o + col0:o + 512],
                                           in1=P[:, o + col0:o + 512], op=ALU.add), reads=[RP[i % 2], RPS[hi]], writes=[RPS[hi]])
                S.op("act", I("activation", out=V3(G[:, :])[:, :, col0:512], in_=V3(cc[:, :])[:, :, col0:512], func=AF.Exp, scale=-1.0),
                     reads=Rcc, writes=[RG[i % 2]])

            def stC(i):
                c, kb, col0, diag, first_ = prm(i)
                E, G, A = E_sb[i % 3], G_sb[i % 2], A_sb[0]
                S.op("dve", I("tensor_tensor", out=V3(A[:, :])[:, :, col0:512], in0=V3(E[:, :])[:, :, col0:512],
                              in1=V3(G[:, :])[:, :, col0:512], op=ALU.mult), reads=[RE[i % 3], RG[i % 2]], writes=[RA[0]])
                fns = []
                for hi in range(2):
                    hb = hi * 64
                    h = 2 * c + hi
                    fns.append(I("matmul", banks[bo][hb:hb + 64, col0:512], v_sb[:, kb, h * 64:(h + 1) * 64],
                                 A[:, hi * 512 + col0:hi * 512 + 512], start=first_, stop=(kb == 0), skip_group_check=True))
                S.op("pe", fns, reads=[RA[0], Rbig[16 + kb]], writes=[Rbank[bo]])
                if kb == 0:
                    S.op("act", I("activation", out=y_sb[:, c, :], in_=banks[bo][:, :], func=AF.Copy), reads=[Rbank[bo]], writes=[Ry[c]])

            nf_steps = max(1, n // 4 - 1)
            for i in range(n + 2):
                if i < n:
                    stA1(i)
                    stA2(i)
                if 0 <= i - 1 < n:
                    stB(i - 1)
                if 0 <= i - 2 < n:
                    stC(i - 2)
                if first is not None:
                    k = first.ticks_needed(nf_steps - i) if i < nf_steps else 10 ** 6
                    first.tick(k)
                    if first.done():
                        first = None
                elif side is not None:
                    side.tick(side.ticks_needed(n - i))
                    if side.done():
                        side = None
            if first is not None:
                first.tick(10 ** 6)
            if side is not None:
                side.tick(10 ** 6)

        C0 = 0.7978845608028654
        C1 = 0.044715

        def gelu_gen(bk, out_ap, Rout):
            j = 0
            xs, tt_ = etmp[j], sg_sb[j]
            S.op("act", I("activation", out=xs[:], in_=banks[bk][:, :], func=AF.Copy), reads=[Rbank[bk]], writes=[Retmp[j]])
            yield
            S.op("dve", I("scalar_tensor_tensor", out=tt_[:], in0=xs[:], scalar=C1, in1=xs[:], op0=ALU.mult, op1=ALU.mult),
                 reads=[Retmp[j]], writes=[Rsg[j]])
            yield
            S.op("dve", I("scalar_tensor_tensor", out=tt_[:], in0=tt_[:], scalar=1.0, in1=xs[:], op0=ALU.add, op1=ALU.mult),
                 reads=[Retmp[j], Rsg[j]], writes=[Rsg[j]])
            yield
            S.op("act", I("activation", out=tt_[:], in_=tt_[:], func=AF.Exp, scale=-2.0 * C0), reads=[Rsg[j]], writes=[Rsg[j]])
            yield
            S.op("dve", I("tensor_scalar_add", out=tt_[:], in0=tt_[:], scalar1=1.0), reads=[Rsg[j]], writes=[Rsg[j]])
            yield
            S.op("dve", I("reciprocal", out=tt_[:], in_=tt_[:]), reads=[Rsg[j]], writes=[Rsg[j]])
            yield
            S.op("dve", I("tensor_tensor", out=out_ap, in0=xs[:], in1=tt_[:], op=ALU.mult), reads=[Retmp[j], Rsg[j]], writes=Rout)
            yield

        def gelu_gen2(j, pb, Rpb, out_ap, Rout, native=False):
            if native:
                S.op("act", I("activation", out=out_ap, in_=pb[:, :], func=AF.Gelu_apprx_tanh), reads=Rpb, writes=Rout)
                yield
                return
            xs, tt_ = etmp[j], sg_sb[j]
            S.op("act", I("activation", out=xs[:], in_=pb[:, :], func=AF.Copy), reads=Rpb, writes=[Retmp[j]])
            yield
            S.op("dve", I("scalar_tensor_tensor", out=tt_[:], in0=xs[:], scalar=C1, in1=xs[:], op0=ALU.mult, op1=ALU.mult),
                 reads=[Retmp[j]], writes=[Rsg[j]])
            yield
            S.op("dve", I("scalar_tensor_tensor", out=tt_[:], in0=tt_[:], scalar=1.0, in1=xs[:], op0=ALU.add, op1=ALU.mult),
                 reads=[Retmp[j], Rsg[j]], writes=[Rsg[j]])
            yield
            S.op("act", I("activation", out=tt_[:], in_=tt_[:], func=AF.Exp, scale=-2.0 * C0), reads=[Rsg[j]], writes=[Rsg[j]])
            yield
            S.op("dve", I("tensor_scalar_add", out=tt_[:], in0=tt_[:], scalar1=1.0), reads=[Rsg[j]], writes=[Rsg[j]])
            yield
            S.op("dve", I("reciprocal", out=tt_[:], in_=tt_[:]), reads=[Rsg[j]], writes=[Rsg[j]])
            yield
            S.op("dve", I("tensor_tensor", out=out_ap, in0=xs[:], in1=tt_[:], op=ALU.mult), reads=[Retmp[j], Rsg[j]], writes=Rout)
            yield

        def inproj_lanes(l, blk, native=False):
            w_in_v = w_in_d[l].rearrange("(k p) c -> p k c", p=128)
            qTb, RqTb = qT2[blk % 2], RqT2[blk % 2]
            g3 = lambda ap: ap.rearrange("p (g d) -> p g d", d=64)

            def norm_lane(tts):
                yield from rstd_batch_gen([blk * 4 + tt for tt in tts])
                for tt in tts:
                    yield from norm_gen(blk * 4 + tt, hT, [RhT], tt * 128, 0)

            LB = {0: (banks[0], [Rbank[0]]), 1: (banks[1], [Rbank[1]]), 2: (bank2f, [Rbank[2]])}

            def fm_group(kind, sh, wsl, Rw, cc, j, lb):
                c = sh * 2 + cc
                pb, Rpb = LB[lb]
                S.op("pe", [I("matmul", pb[:, :], wsl[:, k, cc * 128:(cc + 1) * 128], hT[:, k, :],
                              start=(k == 0), stop=(k == 7)) for k in range(8)], reads=[Rw, RhT], writes=Rpb)
                yield
                if kind == "q":
                    S.op("act", I("activation", out=qTb[:, c, :], in_=pb[:, :], func=AF.Copy, scale=0.125),
                         reads=Rpb, writes=[RqTb])
                    yield
                elif kind == "k":
                    S.op("dve", I("tensor_copy", out=kT[:, c, blk * 512:(blk + 1) * 512], in_=pb[:, :]),
                         reads=Rpb, writes=[Rbig[c * 4 + blk]])
                    yield
                else:
                    yield from gelu_gen2(j, pb, Rpb, uT[:, c, :], [RuT], native)

            def tm_group(kind, w0, Rw0, w1, Rw1, tt, j, lb):
                t = blk * 4 + tt
                pb, Rpb = LB[lb]
                fns = []
                for hf, wsl in ((0, w0), (1, w1)):
                    fns += [I("matmul", pb[:, hf * 256:(hf + 1) * 256], hT[:, k, tt * 128:(tt + 1) * 128], wsl[:, k, :],
                              start=(k == 0), stop=(k == 7)) for k in range(8)]
                S.op("pe", fns, reads=[Rw0, Rw1, RhT], writes=Rpb)
                yield
                if kind == "v":
                    S.op("dve", I("tensor_copy", out=v_sb[:, t, :], in_=pb[:, :]), reads=Rpb, writes=[Rbig[16 + t]])
                    yield
                else:
                    yield from gelu_gen2(j, pb, Rpb, gate_n[:, tt, :], [Rgn[tt]], native)
                    S.op("pool", I("tensor_tensor", out=sqb[j][:], in0=gate_n[:, tt, :], in1=gate_n[:, tt, :], op=ALU.mult),
                         reads=[Rgn[tt]], writes=[Rsqb[j]])
                    yield
                    S.op("dve", I("tensor_reduce", out=gss[:, tt * 8:(tt + 1) * 8], in_=g3(sqb[j][:]), axis=AX.X, op=ALU.add),
                         reads=[Rsqb[j]], writes=[Rgst[tt]])
                    yield

            def proj_lane_a():
                for kind, sbase in (("k", 2), ("q", 0)):
                    for sh in range(2):
                        wsl, Rw, hd = ws.next(w_in_v[:, :, (sbase + sh) * 256:(sbase + sh + 1) * 256], [128, 8, 256])
                        for cc in range(2):
                            yield from fm_group(kind, sh, wsl, Rw, cc, 0, 2)
                        ws.release(hd)
                w0, Rw0, h0 = ws.next(w_in_v[:, :, 4 * 256:5 * 256], [128, 8, 256])
                w1, Rw1, h1 = ws.next(w_in_v[:, :, 5 * 256:6 * 256], [128, 8, 256])
                for tt in range(4):
                    yield from tm_group("v", w0, Rw0, w1, Rw1, tt, 0, 2)
                ws.release(h0)
                ws.release(h1)

            def proj_lane_b():
                slc = []
                hds = []
                for sh in range(2):
                    a_, b_, c_ = ws.next(w_in_v[:, :, (6 + sh) * 256:(7 + sh) * 256], [128, 8, 256])
                    slc.append((a_, b_))
                    hds.append(c_)
                w0, Rw0, h0 = ws.next(w_in_v[:, :, 8 * 256:9 * 256], [128, 8, 256])
                w1, Rw1, h1 = ws.next(w_in_v[:, :, 9 * 256:10 * 256], [128, 8, 256])
                hds += [h0, h1]
                gens = []
                for sh in range(2):
                    for cc in range(2):
                        gens.append(("u", sh, cc))
                for tt in range(4):
                    gens.append(("g", tt, 0))
                subs = [[], []]
                for idx, it in enumerate(gens):
                    subs[idx % 2].append(it)

                def sub(j):
                    for it in subs[j]:
                        if it[0] == "u":
                            wsl, Rw = slc[it[1]]
                            yield from fm_group("u", it[1], wsl, Rw, it[2], j, j)
                        else:
                            yield from tm_group("g", w0, Rw0, w1, Rw1, it[1], j, j)
                a, b = sub(0), sub(1)
                alive = [a, b]
                while alive:
                    nxt = []
                    for g in alive:
                        if next(g, "END") != "END":
                            nxt.append(g)
                    alive = nxt
                    yield
                for hd in hds:
                    ws.release(hd)

            def gate_fin_lane(tts):
                for tt in tts:
                    S.op("act", I("activation", out=glv[:, tt * 8:(tt + 1) * 8], in_=gss[:, tt * 8:(tt + 1) * 8], func=AF.Ln, bias=EPS,
                                  scale=1.0 / 64), reads=[Rgst[tt]], writes=[Rgst[tt]])
                    yield
                    S.op("act", I("activation", out=grs[:, tt * 8:(tt + 1) * 8], in_=glv[:, tt * 8:(tt + 1) * 8], func=AF.Exp, scale=-0.5),
                         reads=[Rgst[tt]], writes=[Rgst[tt]])
                    yield
                    S.op("dve", I("tensor_tensor", out=g3(gate_n[:, tt, :]), in0=g3(gate_n[:, tt, :]),
                                  in1=grs[:, tt * 8:(tt + 1) * 8].unsqueeze(2).broadcast_to([128, 8, 64]), op=ALU.mult),
                         reads=[Rgn[tt], Rgst[tt]], writes=[Rgn[tt]])
                    yield
                    S.op("dve", I("tensor_tensor", out=gate_n[:, tt, :], in0=gate_n[:, tt, :], in1=gsgu_bc[:], op=ALU.mult),
                         reads=[Rgn[tt], Rgsgu], writes=[Rgn[tt]])
                    yield

            return [[norm_lane((0, 1, 2, 3))], [proj_lane_a(), proj_lane_b()], [gate_fin_lane((0, 2)), gate_fin_lane((1, 3))]]

        def sgu_lanes(l, blk):
            def lane():
                for tt in range(4):
                    bk = acc_bank()
                    S.op("pe", [I("matmul", banks[bk][(g % 2) * 64:(g % 2) * 64 + 64, (g // 2) * 128:(g // 2 + 1) * 128],
                                  gate_n[:, tt, g * 64:(g + 1) * 64], WsT[:, l, g, :], start=True, stop=True, skip_group_check=True)
                                for g in range(8)], reads=[Rgn[tt], RWs], writes=[Rbank[bk]])
                    yield
                    S.op("dve", I("tensor_tensor", out=stmp[:], in0=banks[bk][:, :], in1=B_sgu[:].rearrange("p j t -> p (j t)"),
                                  op=ALU.add), reads=[Rbank[bk], RB], writes=[Rstmp])
                    yield
                    S.op("dve", I("tensor_tensor", out=y_sb[:, 4:8, tt * 128:(tt + 1) * 128],
                                  in0=stmp[:].rearrange("p (j t) -> p j t", t=128), in1=uT[:, :, tt * 128:(tt + 1) * 128], op=ALU.mult),
                         reads=[Rstmp, RuT], writes=Ry[4:8])
                    yield
            return [[lane()]]

        def ynorm_outproj_lanes(l, blk):
            w_out_v = w_out_d[l].rearrange("(k p) c -> p k c", p=128)

            def ynorm_lane(i):
                for c in range(i, 8, 2):
                    S.op("pool", I("tensor_tensor", out=sqb[i][:], in0=y_sb[:, c, :], in1=y_sb[:, c, :], op=ALU.mult),
                         reads=[Ry[c]], writes=[Rsqb[i]])
                    yield
                    bk = i
                    S.op("pe", I("matmul", banks[bk][:, :], bones[:], sqb[i][:], start=True, stop=True),
                         reads=[Rsqb[i], Rconst], writes=[Rbank[bk]])
                    yield
                    S.op("act", I("activation", out=nl[i][:], in_=banks[bk][:, :], func=AF.Ln, bias=EPS, scale=1.0 / 64),
                         reads=[Rbank[bk]], writes=[Rnl[i]])
                    yield
                    S.op("act", I("activation", out=nl[i][:], in_=nl[i][:], func=AF.Exp, scale=-0.5), reads=[Rnl[i]], writes=[Rnl[i]])
                    yield
                    S.op("dve", I("scalar_tensor_tensor", out=y_sb[:, c, :], in0=y_sb[:, c, :], scalar=gout_c[:, c:c + 1],
                                  in1=nl[i][:], op0=ALU.mult, op1=ALU.mult), reads=[Ry[c], Rgout, Rnl[i]], writes=[Ry[c]])
                    yield

            def outproj_lane():
                for hf in range(2):
                    w0, Rw0, h0 = ws.next(w_out_v[:, :, (2 * hf) * 256:(2 * hf + 1) * 256], [128, 8, 256])
                    w1, Rw1, h1 = ws.next(w_out_v[:, :, (2 * hf + 1) * 256:(2 * hf + 2) * 256], [128, 8, 256])
                    for tt in range(4):
                        t = blk * 4 + tt
                        bk = acc_bank()
                        fns = []
                        for q_, wsl in ((0, w0), (1, w1)):
                            fns += [I("matmul", banks[bk][:, q_ * 256:(q_ + 1) * 256], y_sb[:, k, tt * 128:(tt + 1) * 128], wsl[:, k, :],
                                      start=(k == 0), stop=(k == 7)) for k in range(8)]
                        S.op("pe", fns, reads=[Rw0, Rw1] + Ry, writes=[Rbank[bk]])
                        yield
                        xs = x_sb[:, t, hf * 512:(hf + 1) * 512]
                        S.op("dve", I("tensor_tensor", out=xs, in0=xs, in1=banks[bk][:, :], op=ALU.add),
                             reads=[Rbank[bk], Rx[t]], writes=[Rx[t]])
                        yield
                    ws.release(h0)
                    ws.release(h1)
            return [[ynorm_lane(0), ynorm_lane(1)], [outproj_lane()]]

        def chain(*gens):
            for g in gens:
                yield from g

        def run(g):
            for _ in g:
                pass

        def phaseA(l, pre=None):
            load_gain(g_mix_d[l])
            S.dma("sp", I("dma_start", out=gsgu_bc[:], in_=g_sgu_d[l].partition_broadcast(128)), writes=[Rgsgu])
            for hh in range(2):
                S.dma("sp", I("dma_start", out=B_sgu[hh * 64:(hh + 1) * 64, :, :],
                              in_=sgu_b_d[l].rearrange("(j two) t -> two j t", two=2)[hh].partition_broadcast(64)), writes=[RB])
            S.dma("sp", I("dma_start", out=gout_c[:], in_=g_out_d[l].rearrange("(c p) -> p c", p=128),
                          allow_slow_non_contiguous=True), writes=[Rgout])
            if pre:
                for f in pre:
                    f()
            Lanes(inproj_lanes(l, 0, True) + sgu_lanes(l, 0), 1).tick(10 ** 6)
            for blk in range(4):
                first = None
                stages = []
                if blk > 0:
                    first = Lanes(ynorm_outproj_lanes(l, blk - 1), 36)
                    stages += sgu_lanes(l, blk)
                if blk < 3:
                    stages += inproj_lanes(l, blk + 1)
                side = Lanes(stages, (12 if blk > 0 else 0) + (85 if blk < 3 else 0)) if stages else None
                attention_block(blk, side, first)
            Lanes(ynorm_outproj_lanes(l, 3), 1).tick(10 ** 6)

        pending = []
        dacc = [0]
        DBANKS = (0, 1, 7)

        def ffn_core(wg_v, wu_v, wd_rows, nch, gate_col):
            wgs, Rwg, hg = ws.next(wg_v, [128, 8, nch * 128])
            wus, Rwu, hu = ws.next(wu_v, [128, 8, nch * 128])
            wds, Rwd, hdn = ws.next(wd_rows.rearrange("(fc p) d -> p fc d", p=128), [128, nch, D])

            def gu_parts(tb, ai):
                hsegs = [Rbig[k * 4 + tb] for k in range(8)]
                parts = []
                for fc in range(nch):
                    st_ = {}

                    def g_part(fc=fc, st_=st_):
                        bg = 3 + (cnt["z"] % 2)
                        cnt["z"] += 1
                        st_["bg"] = bg
                        S.op("pe", [I("matmul", banks[bg][:, :], wgs[:, k, fc * 128:(fc + 1) * 128], h2T[:, k, tb * 512:(tb + 1) * 512],
                                      start=(k == 0), stop=(k == 7)) for k in range(8)], reads=[Rwg] + hsegs, writes=[Rbank[bg]])
                        S.op("act", I("activation", out=sg_sb[fc % 2][:], in_=banks[bg][:, :], func=AF.Silu),
                             reads=[Rbank[bg]], writes=[Rsg[fc % 2]])

                    def u_part(fc=fc, st_=st_):
                        bu = 5 + (cnt["c"] % 2)
                        cnt["c"] += 1
                        S.op("pe", [I("matmul", banks[bu][:, :], wus[:, k, fc * 128:(fc + 1) * 128], h2T[:, k, tb * 512:(tb + 1) * 512],
                                      start=(k == 0), stop=(k == 7)) for k in range(8)], reads=[Rwu] + hsegs, writes=[Rbank[bu]])
                        S.op("dve", I("tensor_tensor", out=aT[ai][:, fc, :], in0=sg_sb[fc % 2][:], in1=banks[bu][:, :], op=ALU.mult),
                             reads=[Rsg[fc % 2], Rbank[bu]], writes=[RaT[ai]])
                    parts += [g_part, u_part]
                return parts

            def down_parts(tb, ai):
                parts = []
                for tt in range(4):
                    for hf in range(2):
                        def d_part(tt=tt, hf=hf):
                            t = tb * 4 + tt
                            bk = DBANKS[dacc[0] % 3]
                            dacc[0] += 1
                            S.op("pe", [I("matmul", banks[bk][:, :], aT[ai][:, fc, tt * 128:(tt + 1) * 128], wds[:, fc, hf * 512:(hf + 1) * 512],
                                          start=(fc == 0), stop=(fc == nch - 1)) for fc in range(nch)],
                                 reads=[Rwd, RaT[ai]], writes=[Rbank[bk]])
                            xs = x_sb[:, t, hf * 512:(hf + 1) * 512]
                            if gate_col is None:
                                S.op("dve", I("tensor_tensor", out=xs, in0=xs, in1=banks[bk][:, :], op=ALU.add),
                                     reads=[Rbank[bk], Rx[t]], writes=[Rx[t]])
                            else:
                                S.op("dve", I("scalar_tensor_tensor", out=xs, in0=banks[bk][:, :], scalar=gates[:, t, gate_col:gate_col + 1],
                                              in1=xs, op0=ALU.mult, op1=ALU.add), reads=[Rbank[bk], Rx[t], Rgates[t]], writes=[Rx[t]])
                        parts.append(d_part)
                return parts

            for tb in range(4):
                ai = cnt["u"] % 2
                cnt["u"] += 1
                gp = gu_parts(tb, ai)
                dp = list(pending)
                del pending[:]
                per = (len(dp) + len(gp) - 1) // len(gp)
                for g in gp:
                    g()
                    for _ in range(per):
                        if dp:
                            dp.pop(0)()
                while dp:
                    dp.pop(0)()
                pending.extend(down_parts(tb, ai))
            ws.release(hg)
            ws.release(hu)
            pending.append(lambda: ws.release(hdn))

        def ffn_flush():
            while pending:
                pending.pop(0)()

        def router_tile(t):
            bk = acc_bank()
            S.op("pe", [I("matmul", banks[bk][:, 0:NE], h2T[:, k, t * 128:(t + 1) * 128], router_sb[:, k, :],
                          start=(k == 0), stop=(k == 7)) for k in range(8)],
                 reads=[Rrouter] + [Rbig[k * 4 + t // 4] for k in range(8)], writes=[Rbank[bk]])
            L = dict(reads=[Rlog], writes=[Rlog])
            S.op("dve", I("tensor_copy", out=logit[:], in_=banks[bk][:, 0:NE]), reads=[Rbank[bk]], writes=[Rlog])
            S.op("dve", I("tensor_reduce", out=m1[:, 0:1], in_=logit[:], axis=AX.X, op=ALU.max), **L)
            S.op("dve", I("tensor_scalar", out=mk1[:], in0=logit[:], scalar1=m1[:, 0:1], scalar2=None, op0=ALU.is_equal), **L)
            S.op("dve", I("scalar_tensor_tensor", out=l2[:], in0=mk1[:], scalar=-1e30, in1=logit[:], op0=ALU.mult, op1=ALU.add), **L)
            S.op("dve", I("tensor_reduce", out=m1[:, 1:2], in_=l2[:], axis=AX.X, op=ALU.max), **L)
            S.op("dve", I("tensor_scalar", out=mk2[:], in0=l2[:], scalar1=m1[:, 1:2], scalar2=None, op0=ALU.is_equal), **L)
            S.op("dve", I("tensor_tensor", out=m1[:, 2:3], in0=m1[:, 1:2], in1=m1[:, 0:1], op=ALU.subtract), **L)
            S.op("act", I("activation", out=m1[:, 2:3], in_=m1[:, 2:3], func=AF.Exp), **L)
            S.op("dve", I("tensor_scalar_add", out=m1[:, 3:4], in0=m1[:, 2:3], scalar1=1.0), **L)
            S.op("dve", I("reciprocal", out=m1[:, 3:4], in_=m1[:, 3:4]), **L)
            S.op("dve", I("tensor_tensor", out=m1[:, 2:3], in0=m1[:, 2:3], in1=m1[:, 3:4], op=ALU.mult), **L)
            S.op("dve", I("tensor_scalar", out=mk1[:], in0=mk1[:], scalar1=m1[:, 3:4], scalar2=None, op0=ALU.mult), **L)
            S.op("dve", I("scalar_tensor_tensor", out=gates[:, t, :], in0=mk2[:], scalar=m1[:, 2:3], in1=mk1[:],
                          op0=ALU.mult, op1=ALU.add), reads=[Rlog], writes=[Rgates[t]])

        def phaseB(l):
            load_gain(g_ffn_d[l])
            for q4 in range(4):
                run(rstd_batch_gen(list(range(q4 * 4, q4 * 4 + 4))))
            for t in range(NT):
                norm_tile(t, h2T, [Rbig[k * 4 + t // 4] for k in range(8)], t * 128)
            i = l // 2
            if l % 2 == 0:
                wg_v = ffn_wg_d[i].rearrange("(k p) c -> p k c", p=128)
                wu_v = ffn_wu_d[i].rearrange("(k p) c -> p k c", p=128)
                for fg in range(D_FF // 256):
                    ffn_core(wg_v[:, :, fg * 256:(fg + 1) * 256], wu_v[:, :, fg * 256:(fg + 1) * 256],
                             ffn_wd_d[i][fg * 256:(fg + 1) * 256, :], 2, None)
                ffn_flush()
            else:
                for t in range(NT):
                    router_tile(t)
                for ex in range(NE):
                    wg_v = moe_wg_d[i, ex].rearrange("(k p) c -> p k c", p=128)
                    wu_v = moe_wu_d[i, ex].rearrange("(k p) c -> p k c", p=128)
                    f0 = 0
                    while f0 < D_FFE:
                        nch = min(2, (D_FFE - f0) // 128)
                        ffn_core(wg_v[:, :, f0:f0 + nch * 128], wu_v[:, :, f0:f0 + nch * 128],
                                 moe_wd_d[i, ex][f0:f0 + nch * 128, :], nch, ex)
                        f0 += nch * 128
                ffn_flush()

        fin = []

        def program():
            setup_consts()
            for s in range(nseq):
                for t in range(4):
                    S.dma("sp", I("dma_start", out=x_sb[:, t, :], in_=x_d[s, t * 128:(t + 1) * 128, :]), writes=[Rx[t]])
                rest = [(lambda t=t, s=s: S.dma("sp", I("dma_start", out=x_sb[:, t, :], in_=x_d[s, t * 128:(t + 1) * 128, :]),
                                               writes=[Rx[t]])) for t in range(4, NT)]
                done = False
                for l in range(depth):
                    phaseA(l, rest if l == 0 else None)
                    if stop == "A%d" % l:
                        done = True
                        break
                    phaseB(l)
                    if stop == "F%d" % l:
                        done = True
                        break
                if not done:
                    load_gain(g_final_d)
                    for q4 in range(4):
                        run(rstd_batch_gen(list(range(q4 * 4, q4 * 4 + 4))))
                    for t in range(NT):
                        S.op("dve", I("scalar_tensor_tensor", out=x_sb[:, t, :], in0=x_sb[:, t, :], scalar=rstd[:, t:t + 1],
                                      in1=gain_bc[:], op0=ALU.mult, op1=ALU.mult), reads=[Rx[t], Rss[t], Rgain], writes=[Rx[t]])
                for t in range(NT):
                    stp = S.dma("sp", I("dma_start", out=out_d[s, t * 128:(t + 1) * 128, :], in_=x_sb[:, t, :]), reads=[Rx[t]])
                    if stp is not None:
                        fin.append(stp)

        S.dry = True
        ws.record = True
        program()
        S.dry = False
        ws.start_real()
        for k in cnt:
            cnt[k] = 0
        acc_rr[0] = 0
        dacc[0] = 0
        program()
        S.final_wait("sp", fin)
        print("sbuf remaining", nc.sbuf_bytes_remaining, "ops", S.n_ins, "waits", S.n_wait, "wslices", len(ws.plan))
        with nc.Block() as block:
            S.emit(block)
    return nc


_WEIGHT_NAMES = ["w_in", "w_out", "g_mix", "g_ffn", "g_sgu", "sgu_w", "sgu_b", "g_out", "ffn_w_gate", "ffn_w_up",
                 "ffn_w_down", "router_w", "moe_w_gate", "moe_w_up", "moe_w_down", "g_final"]


def kernel(**inputs):
    x = np.ascontiguousarray(np.asarray(inputs["x"], dtype=np.float32))
    wts = {n: np.ascontiguousarray(np.asarray(inputs[n], dtype=np.float32)) for n in _WEIGHT_NAMES}
    nc = build()
    in_maps = []
    for c in range(NCORES):
        m = {"x": x[c * SEQ_PER_CORE:(c + 1) * SEQ_PER_CORE]}
        m.update(wts)
        in_maps.append(m)
    res = run_bass_kernel_spmd(nc, in_maps, core_ids=list(range(NCORES)))
    out = np.concatenate([np.asarray(r["out"]) for r in res.results], axis=0)
    return out.astype(np.float32, copy=False)
```

```python
import contextlib
from collections import deque

import numpy as np
import concourse.bass as bass
import concourse.mybir as mybir
from concourse.bass_utils import run_bass_kernel_spmd

F32 = mybir.dt.float32
BF16 = mybir.dt.bfloat16
AF = mybir.ActivationFunctionType
ALU = mybir.AluOpType
AX = mybir.AxisListType

D = 1024
SEQ = 2048
NT = SEQ // 128
DEPTH = 2
D_IN = 2560
D_FF = 2816
NE = 8
D_FFE = 1408
EPS = 1e-6
NCORES = 8
SEQ_PER_CORE = 4
NSLOT = 8


def I(name, *a, **kw):
    return lambda e: getattr(e, name)(*a, **kw)


class R:
    __slots__ = ("w", "r")

    def __init__(self):
        self.w = None
        self.r = {}


class Sched:
    ENGS = ("pe", "act", "dve", "pool", "sp")

    def __init__(self, nc, stack, n_dma_sems=32, n_fresh=0):
        self.nc = nc
        self.dry = False
        self.fresh = [stack.enter_context(nc.semaphore("s_fr%d" % i)) for i in range(n_fresh)]
        self.fresh_i = 0
        self.q = {e: [] for e in self.ENGS}
        self.sems = {}
        self.count = {}
        for e in self.ENGS:
            self.sems[e] = stack.enter_context(nc.semaphore("s_" + e))
            self.count[e] = 0
        self.n_dma = n_dma_sems
        for i in range(n_dma_sems):
            k = ("dma", i)
            self.sems[k] = stack.enter_context(nc.semaphore("s_dma%d" % i))
            self.count[k] = 0
        self.dma_rr = 0
        self.n_pdma = 16
        for i in range(self.n_pdma):
            k = ("pdma", i)
            self.sems[k] = stack.enter_context(nc.semaphore("s_pdma%d" % i))
            self.count[k] = 0
        self.pdma_rr = 0
        self.seen = {e: {} for e in self.ENGS}
        self.n_wait = 0
        self.n_ins = 0

    def _waits(self, eng, reads, writes, extra=()):
        need = {}

        def add(st):
            if st is None:
                return
            k, v = st
            if need.get(k, 0) < v:
                need[k] = v

        for r in reads:
            add(r.w)
        for w in writes:
            add(w.w)
            for k, v in w.r.items():
                add((k, v))
        for st in extra:
            add(st)
        out = []
        seen = self.seen[eng]
        for k, v in need.items():
            if seen.get(k, 0) >= v:
                continue
            seen[k] = v
            out.append((self.sems[k], v))
        return out

    def _mark(self, stamp, reads, writes):
        k, v = stamp
        for w in writes:
            w.w = stamp
            w.r = {}
        for r in reads:
            if r.r.get(k, 0) < v:
                r.r[k] = v

    def op(self, eng, fns, reads=(), writes=()):
        if self.dry:
            return None
        if callable(fns):
            fns = [fns]
        waits = self._waits(eng, reads, writes)
        self.count[eng] += 1
        stamp = (eng, self.count[eng])
        sem = self.sems[eng]
        self.n_wait += len(waits)
        self.n_ins += len(fns)

        def thunk(e, waits=waits, fns=fns, sem=sem):
            for s, v in waits:
                e.wait_ge(s, v)
            last = None
            for f in fns:
                last = f(e)
            last.then_inc(sem, 1)

        self.q[eng].append(thunk)
        self._mark(stamp, reads, writes)
        return stamp

    def dma(self, eng, fn, reads=(), writes=()):
        if self.dry:
            return None
        if eng == "pool" and self.fresh:
            k = ("fresh", self.fresh_i)
            self.sems[k] = self.fresh[self.fresh_i]
            self.count[k] = 0
            self.fresh_i += 1
        elif eng == "pool":
            i = self.pdma_rr
            self.pdma_rr = (self.pdma_rr + 1) % self.n_pdma
            k = ("pdma", i)
        else:
            i = self.dma_rr
            self.dma_rr = (self.dma_rr + 1) % self.n_dma
            k = ("dma", i)
        prev = (k, self.count[k]) if self.count[k] else None
        waits = self._waits(eng, reads, writes, extra=(prev,) if prev else ())
        self.count[k] += 16
        stamp = (k, self.count[k])
        sem = self.sems[k]
        self.n_wait += len(waits)
        self.n_ins += 1

        def thunk(e, waits=waits, fn=fn, sem=sem):
            for s, v in waits:
                e.wait_ge(s, v)
            fn(e).then_inc(sem, 16)

        self.q[eng].append(thunk)
        self._mark(stamp, reads, writes)
        return stamp

    def final_wait(self, eng, stamps):
        need = {}
        for k, v in stamps:
            if need.get(k, 0) < v:
                need[k] = v
        ws = [(self.sems[k], v) for k, v in need.items()]

        def thunk(e, ws=ws):
            for s, v in ws:
                e.wait_ge(s, v)

        self.q[eng].append(thunk)

    def emit(self, block):
        q = self.q

        @block.tensor
        def _(e):
            for t in q["pe"]:
                t(e)

        @block.scalar
        def _(e):
            for t in q["act"]:
                t(e)

        @block.vector
        def _(e):
            for t in q["dve"]:
                t(e)

        @block.gpsimd
        def _(e):
            for t in q["pool"]:
                t(e)

        @block.sync
        def _(e):
            for t in q["sp"]:
                t(e)


class Lanes:
    def __init__(self, stages, est_ticks):
        self.stages = [list(st) for st in stages if st]
        self.est = max(1, est_ticks)
        self.used = 0

    def done(self):
        return not self.stages

    def tick(self, k=1):
        for _ in range(k):
            if not self.stages:
                return
            self.used += 1
            lanes = self.stages[0]
            alive = []
            for g in lanes:
                if next(g, "END") != "END":
                    alive.append(g)
            if alive:
                self.stages[0] = alive
            else:
                self.stages.pop(0)

    def ticks_needed(self, steps_left):
        rem = max(1, self.est - self.used)
        return max(1, (rem + max(1, steps_left) - 1) // max(1, steps_left))


class WStream:
    def __init__(self, S, slots, nslot):
        self.S = S
        self.slots = slots
        self.R = [R() for _ in range(nslot)]
        self.nslot = nslot
        self.plan = []
        self.record = True
        self.i = 0
        self.issued = 0
        self.free = list(range(nslot))
        self.slot_of = {}

    def start_real(self):
        self.record = False
        self.i = 0
        self.issued = 0
        self.free = list(range(self.nslot))
        self.slot_of = {}

    def _view(self, k, shape):
        n = 1
        for s_ in shape[1:]:
            n *= s_
        v = self.slots[k][:, 0:n]
        if len(shape) == 3:
            v = v.rearrange("p (a b) -> p a b", b=shape[2])
        return v

    def _prefetch(self):
        while self.issued < len(self.plan) and self.free and self.issued - self.i < 4:
            j = self.issued
            k = self.free.pop(0)
            src, shape = self.plan[j]
            dst = self._view(k, shape)
            self.S.dma("pool", lambda e, dst=dst, src=src: e.dma_start(out=dst, in_=src), writes=[self.R[k]])
            self.slot_of[j] = k
            self.issued += 1

    def next(self, src, shape):
        i = self.i
        self.i += 1
        if self.record:
            self.plan.append((src, shape))
            return self._view(0, shape), self.R[0], None
        self._prefetch()
        assert i in self.slot_of, "weight ring exhausted: too many live slices"
        k = self.slot_of[i]
        return self._view(k, shape), self.R[k], (i, k)

    def release(self, handle):
        if handle is None:
            return
        i, k = handle
        self.free.append(k)
        self._prefetch()


def build(nseq=SEQ_PER_CORE, depth=DEPTH, stop=None, n_fresh=0, ndummy=0):
    nc = bass.Bass("TRN2", target_bir_lowering=False)
    dt = lambda name, shape: nc.dram_tensor(name, shape, F32, kind="ExternalInput").ap()
    x_d = dt("x", [nseq, SEQ, D])
    w_in_d = dt("w_in", [DEPTH, D, D_IN])
    w_out_d = dt("w_out", [DEPTH, D, D])
    g_mix_d = dt("g_mix", [DEPTH, D])
    g_ffn_d = dt("g_ffn", [DEPTH, D])
    g_sgu_d = dt("g_sgu", [DEPTH, 512])
    sgu_w_d = dt("sgu_w", [DEPTH, 8, 128, 128])
    sgu_b_d = dt("sgu_b", [DEPTH, 8, 128])
    g_out_d = dt("g_out", [DEPTH, D])
    ffn_wg_d = dt("ffn_w_gate", [1, D, D_FF])
    ffn_wu_d = dt("ffn_w_up", [1, D, D_FF])
    ffn_wd_d = dt("ffn_w_down", [1, D_FF, D])
    router_d = dt("router_w", [1, D, NE])
    moe_wg_d = dt("moe_w_gate", [1, NE, D, D_FFE])
    moe_wu_d = dt("moe_w_up", [1, NE, D, D_FFE])
    moe_wd_d = dt("moe_w_down", [1, NE, D_FFE, D])
    g_final_d = dt("g_final", [D])
    out_d = nc.dram_tensor("out", [nseq, SEQ, D], F32, kind="ExternalOutput").ap()

    with contextlib.ExitStack() as st:
        T = lambda name, shape, dtype=F32: st.enter_context(nc.sbuf_tensor(name, shape, dtype))
        x_sb = T("x_sb", [128, NT, D])
        big = T("big", [128, 16384], BF16)
        kT = big[:, 0:8192].rearrange("p (c t) -> p c t", t=SEQ)
        v_sb = big[:, 8192:16384].rearrange("p (t d) -> p t d", d=512)
        h2T = big[:, :].rearrange("p (k t) -> p k t", t=SEQ)
        Rbig = [R() for _ in range(32)]
        hT = T("hT", [128, 8, 512], BF16)
        qT2 = [T("qT%d" % i, [128, 4, 512], BF16) for i in range(2)]
        qT = qT2[0]
        uT = T("uT", [128, 4, 512], BF16)
        gate_n = T("gate_n", [128, 4, 512], BF16)
        y_sb = T("y_sb", [128, 8, 512], BF16)
        slots = [T("wslot%d" % i, [128, 2048], BF16) for i in range(NSLOT)]
        E_sb = [T("E%d" % i, [128, 1024], BF16) for i in range(3)]
        P_sb = [T("P%d" % i, [128, 1024], BF16) for i in range(2)]
        G_sb = [T("G%d" % i, [128, 1024], BF16) for i in range(2)]
        A_sb = [T("A0", [128, 1024], BF16)]
        PS_sb = T("PS", [128, 1024], BF16)
        xn0 = T("xn0", [128, D], BF16)
        xn = [xn0, xn0]
        junk = T("junk", [128, D], BF16)
        ss = T("ss", [128, NT])
        lnv = T("lnv", [128, NT])
        rstd = T("rstd", [128, NT])
        gss = T("gss", [128, 32])
        glv = T("glv", [128, 32])
        grs = T("grs", [128, 32])
        sqb = [T("sqb%d" % i, [128, 512], BF16) for i in range(2)]
        sg_sb = [T("sg%d" % i, [128, 512]) for i in range(2)]
        aT = [qT[:, 0:2, :], uT[:, 0:2, :]]
        etmp = [T("etmp%d" % i, [128, 512]) for i in range(2)]
        stmp = etmp[0]
        nl = [etmp[1], sg_sb[1]]
        ident = T("ident", [128, 128], BF16)
        tri = T("tri", [128, 128], BF16)
        ones = T("ones", [128, 128], BF16)
        bones = T("bones", [128, 128], BF16)
        mask = T("mask", [128, 128], BF16)
        cf = T("cf", [128, 128])
        gain_bc = T("gain_bc", [128, D])
        gsgu_bc = T("gsgu_bc", [128, 512])
        B_sgu = T("B_sgu", [128, 4, 128])
        gout_c = T("gout_c", [128, 8])
        WsT = T("WsT", [128, DEPTH, 8, 128], BF16)
        wtmp = gain_bc[:, :].rearrange("p (g t) -> p g t", t=128)
        wtmpb = junk[:, :].rearrange("p (g t) -> p g t", t=128)
        router_sb = T("router_sb", [128, 8, NE], BF16)
        logit = T("logit", [128, NE])
        m1 = T("m1", [128, 4])
        mk1 = T("mk1", [128, NE])
        mk2 = T("mk2", [128, NE])
        l2 = T("l2", [128, NE])
        gates = T("gates", [128, NT, NE])

        banks = []
        for i in range(3):
            if i == 2:
                banks.append(st.enter_context(nc.psum_tensor("bank2", [128, 1024], BF16)))
            else:
                banks.append(st.enter_context(nc.psum_tensor("bank%d" % i, [128, 512], F32)))
        zz = st.enter_context(nc.psum_tensor("zz", [128, 1024], F32))
        cc = st.enter_context(nc.psum_tensor("cc", [128, 1024], F32))
        banks += [zz[:, 0:512], zz[:, 512:1024], cc[:, 0:512], cc[:, 512:1024]]
        banks.append(st.enter_context(nc.psum_tensor("bank7", [128, 512], F32)))
        pst = banks[2]
        bank2f = banks[2][:, :].bitcast(F32)
        Rbank = [R() for _ in range(8)]
        Rpst = [Rbank[2], Rbank[2]]
        S = Sched(nc, st, n_dma_sems=(8 if n_fresh else 24), n_fresh=n_fresh)
        ws = WStream(S, slots, NSLOT)

        Rx = [R() for _ in range(NT)]
        RhT = R(); RqT2 = [R(), R()]; RqT = RqT2[0]; RuT = R(); Rgn = [R() for _ in range(4)]
        Ry = [R() for _ in range(8)]
        RE = [R(), R(), R()]; RP = [R(), R()]; RG = [R(), R()]; RA = [R()]; RPS = [R(), R()]
        Rxn0 = R(); Rxn = [Rxn0, Rxn0]; Rjunk = R(); Rss = [R() for _ in range(NT)]
        Rgst = [R() for _ in range(4)]
        Rsqb = [R(), R()]; Rsg = [R(), R()]; RaT = [RqT, RuT]; Retmp = [R(), R()]; Rstmp = Retmp[0]; Rnl = [Retmp[1], Rsg[1]]
        Rconst = R(); Rgain = R(); Rgsgu = R(); RB = R(); Rgout = R(); RWs = R(); Rwtmp = Rgain; Rwtmpb = Rjunk; Rcf = R()
        Rrouter = R(); Rlog = R(); Rgates = [R() for _ in range(NT)]

        acc_rr = [0]

        def acc_bank():
            b = acc_rr[0]
            acc_rr[0] = (b + 1) % 2
            return b

        cnt = {"z": 0, "c": 0, "u": 0, "ev": 0, "et": 0}

        def setup_consts():
            def build_mask(dst, pattern, cmp_op, cm):
                S.op("pool", I("memset", cf[:], 1.0), writes=[Rcf])
                S.op("pool", I("affine_select", out=cf[:], in_=cf[:], pattern=pattern, compare_op=cmp_op,
                               fill=0.0, base=0, channel_multiplier=cm), reads=[Rcf], writes=[Rcf])
                S.op("dve", I("tensor_copy", out=dst[:], in_=cf[:]), reads=[Rcf], writes=[Rconst])
            build_mask(ident, [[-1, 128]], ALU.is_equal, 1)
            build_mask(tri, [[-1, 128]], ALU.is_ge, 1)
            S.op("pool", I("memset", cf[:], -30000.0), writes=[Rcf])
            S.op("pool", I("affine_select", out=cf[:], in_=cf[:], pattern=[[-1, 128]], compare_op=ALU.is_ge,
                           fill=0.0, base=0, channel_multiplier=1), reads=[Rcf], writes=[Rcf])
            S.op("dve", I("tensor_copy", out=mask[:], in_=cf[:]), reads=[Rcf], writes=[Rconst])
            S.op("dve", [I("memset", ones[:], 1.0), I("memset", bones[:], 0.0)], writes=[Rconst])
            S.op("dve", [I("memset", bones[0:64, 0:64], 1.0), I("memset", bones[64:128, 64:128], 1.0)], writes=[Rconst])
            for l in range(depth):
                S.dma("sp", I("dma_start", out=wtmp[:], in_=sgu_w_d[l].rearrange("g t s -> t g s")), writes=[Rwtmp])
                S.op("dve", I("tensor_copy", out=wtmpb[:], in_=wtmp[:]), reads=[Rwtmp], writes=[Rwtmpb])
                S.op("pe", [I("transpose", out=pst[:, g * 128:(g + 1) * 128], in_=wtmpb[:, g, :], identity=ident[:])
                            for g in range(8)], reads=[Rwtmpb, Rconst], writes=[Rbank[2]])
                S.op("dve", I("tensor_copy", out=wtmpb[:].rearrange("p g t -> p (g t)"), in_=pst[:, 0:1024]),
                     reads=[Rbank[2]], writes=[Rwtmpb])
                S.op("pool", [I("affine_select", out=WsT[:, l, g, :], in_=wtmpb[:, g, :], pattern=[[1, 128]],
                                compare_op=ALU.is_ge, fill=0.0, base=0, channel_multiplier=-1) for g in range(8)],
                     reads=[Rwtmpb], writes=[RWs])
            if depth > 1:
                S.dma("pool", I("dma_start", out=router_sb[:], in_=router_d[0].rearrange("(k p) e -> p k e", p=128)),
                      writes=[Rrouter])

        def rstd_batch_gen(tiles):
            t0, t1 = tiles[0], tiles[-1] + 1
            Rs = [Rss[t] for t in tiles]
            S.op("pool", I("memset", ss[:, t0:t1], 0.0), writes=Rs)
            yield
            for t in tiles:
                S.op("act", I("activation", out=junk[:], in_=x_sb[:, t, :], func=AF.Square, accum_out=ss[:, t:t + 1]),
                     reads=[Rx[t]], writes=[Rjunk, Rss[t]])
                yield
            S.op("act", I("activation", out=lnv[:, t0:t1], in_=ss[:, t0:t1], func=AF.Ln, bias=EPS, scale=1.0 / D), reads=Rs, writes=Rs)
            yield
            S.op("act", I("activation", out=rstd[:, t0:t1], in_=lnv[:, t0:t1], func=AF.Exp, scale=-0.5), reads=Rs, writes=Rs)
            yield

        def norm_gen(t, dest, Rdest, col0, i=None):
            if i is None:
                i = cnt["u"] % 2
                cnt["u"] += 1
            S.op("dve", I("scalar_tensor_tensor", out=xn[i][:], in0=x_sb[:, t, :], scalar=rstd[:, t:t + 1], in1=gain_bc[:],
                          op0=ALU.mult, op1=ALU.mult), reads=[Rx[t], Rss[t], Rgain], writes=[Rxn[i]])
            yield
            ph = pst[:, i * 512:(i + 1) * 512]
            for half in range(2):
                S.op("pe", [I("transpose", out=ph[:, k * 128:(k + 1) * 128], in_=xn[i][:, (half * 4 + k) * 128:(half * 4 + k + 1) * 128],
                              identity=ident[:]) for k in range(4)], reads=[Rxn[i], Rconst], writes=[Rpst[i]])
                yield
                src = ph.rearrange("p (k t) -> p k t", t=128)
                dst = dest[:, half * 4:(half + 1) * 4, col0:col0 + 128]
                if cnt["ev"] % 2 == 0:
                    S.op("dve", I("tensor_copy", out=dst, in_=src), reads=[Rpst[i]], writes=Rdest)
                else:
                    S.op("act", I("activation", out=dst, in_=src, func=AF.Copy), reads=[Rpst[i]], writes=Rdest)
                cnt["ev"] += 1
                yield

        def norm_tile(t, dest, Rdest, col0):
            for _ in norm_gen(t, dest, Rdest, col0):
                pass

        def load_gain(g_ap):
            S.dma("sp", I("dma_start", out=gain_bc[:], in_=g_ap.partition_broadcast(128)), writes=[Rgain])

        def attention_block(blk, side=None, first=None):
            bo = 7
            qT = qT2[blk % 2]
            RqT = RqT2[blk % 2]
            Rzz = [Rbank[3], Rbank[4]]
            Rcc = [Rbank[5], Rbank[6]]
            units = [(c, kb) for c in range(4) for kb in range(4 * blk + 3, -1, -1)]
            n = len(units)
            V3 = lambda ap: ap.rearrange("p (h q) -> p h q", h=2)

            def prm(i):
                c, kb = units[i]
                col0 = max(0, kb * 128 - blk * 512)
                return c, kb, col0, kb >= 4 * blk, kb == 4 * blk + 3

            def stA1(i):
                c, kb, col0, diag, first_ = prm(i)
                E = E_sb[i % 3]
                fz = []
                for hi in range(2):
                    hb = hi * 64
                    fz.append(I("matmul", zz[:, hi * 512 + col0:hi * 512 + 512], kT[hb:hb + 64, c, kb * 128:(kb + 1) * 128],
                                qT[hb:hb + 64, c, col0:512], start=True, stop=not diag, skip_group_check=True))
                if diag:
                    for hi in range(2):
                        fz.append(I("matmul", zz[:, hi * 512 + col0:hi * 512 + col0 + 128], ident[:], mask[:], start=False, stop=True,
                                    skip_group_check=True))
                S.op("pe", fz, reads=[Rbig[c * 4 + kb // 4], RqT, Rconst], writes=Rzz)
                S.op("act", I("activation", out=V3(E[:, :])[:, :, col0:512], in_=V3(zz[:, :])[:, :, col0:512], func=AF.Exp),
                     reads=Rzz, writes=[RE[i % 3]])

            def stA2(i):
                c, kb, col0, diag, first_ = prm(i)
                E, P = E_sb[i % 3], P_sb[i % 2]
                S.op("act", I("activation", out=V3(P[:, :])[:, :, col0:512], in_=V3(E[:, :])[:, :, col0:512], func=AF.Ln, bias=1.0),
                     reads=[RE[i % 3]], writes=[RP[i % 2]])

            def stB(i):
                c, kb, col0, diag, first_ = prm(i)
                P, G, PS = P_sb[i % 2], G_sb[i % 2], PS_sb
                for hi in range(2):
                    o = hi * 512
                    fns = [I("matmul", cc[:, o + col0:o + 512], tri[:], P[:, o + col0:o + 512], start=True, stop=first_,
                             skip_group_check=True)]
                    if not first_:
                        fns.append(I("matmul", cc[:, o + col0:o + 512], ones[:], PS[:, o + col0:o + 512], start=False, stop=True,
                                     skip_group_check=True))
                    S.op("pe", fns, reads=[RP[i % 2], Rconst] + ([] if first_ else [RPS[hi]]), writes=[Rcc[hi]])
                    if kb > 0:
                        if first_:
                            fl = [I("tensor_copy", out=PS[:, o + col0:o + 512], in_=P[:, o + col0:o + 512])]
                            if col0 > 0:
                                fl.append(I("memset", PS[:, o:o + col0], 0.0))
                            S.op("pool", fl, reads=[RP[i % 2]], writes=[RPS[hi]])
                        else:
                            S.op("pool", I("tensor_tensor", out=PS[:, o + col0:o + 512], in0=PS[:, o + col0:o + 512],
                                           in1=P[:, o + col0:o + 512], op=ALU.add), reads=[RP[i % 2], RPS[hi]], writes=[RPS[hi]])
                S.op("act", I("activation", out=V3(G[:, :])[:, :, col0:512], in_=V3(cc[:, :])[:, :, col0:512], func=AF.Exp, scale=-1.0),
                     reads=Rcc, writes=[RG[i % 2]])

            def stC(i):
                c, kb, col0, diag, first_ = prm(i)
                E, G, A = E_sb[i % 3], G_sb[i % 2], A_sb[0]
                S.op("dve", I("tensor_tensor", out=V3(A[:, :])[:, :, col0:512], in0=V3(E[:, :])[:, :, col0:512],
                              in1=V3(G[:, :])[:, :, col0:512], op=ALU.mult), reads=[RE[i % 3], RG[i % 2]], writes=[RA[0]])
                fns = []
                for hi in range(2):
                    hb = hi * 64
                    h = 2 * c + hi
                    fns.append(I("matmul", banks[bo][hb:hb + 64, col0:512], v_sb[:, kb, h * 64:(h + 1) * 64],
                                 A[:, hi * 512 + col0:hi * 512 + 512], start=first_, stop=(kb == 0), skip_group_check=True))
                S.op("pe", fns, reads=[RA[0], Rbig[16 + kb]], writes=[Rbank[bo]])
                if kb == 0:
                    S.op("act", I("activation", out=y_sb[:, c, :], in_=banks[bo][:, :], func=AF.Copy), reads=[Rbank[bo]], writes=[Ry[c]])

            nf_steps = max(1, n // 4 - 3)
            for i in range(n + 2):
                if i < n:
                    stA1(i)
                    stA2(i)
                if 0 <= i - 1 < n:
                    stB(i - 1)
                if 0 <= i - 2 < n:
                    stC(i - 2)
                if first is not None:
                    k = first.ticks_needed(nf_steps - i) if i < nf_steps else 10 ** 6
                    first.tick(k)
                    if first.done():
                        first = None
                elif side is not None:
                    side.tick(side.ticks_needed(n - i))
                    if side.done():
                        side = None
            if first is not None:
                first.tick(10 ** 6)
            if side is not None:
                side.tick(10 ** 6)

        C0 = 0.7978845608028654
        C1 = 0.044715

        def gelu_gen(bk, out_ap, Rout):
            j = 0
            xs, tt_ = etmp[j], sg_sb[j]
            S.op("act", I("activation", out=xs[:], in_=banks[bk][:, :], func=AF.Copy), reads=[Rbank[bk]], writes=[Retmp[j]])
            yield
            S.op("dve", I("scalar_tensor_tensor", out=tt_[:], in0=xs[:], scalar=C1, in1=xs[:], op0=ALU.mult, op1=ALU.mult),
                 reads=[Retmp[j]], writes=[Rsg[j]])
            yield
            S.op("dve", I("scalar_tensor_tensor", out=tt_[:], in0=tt_[:], scalar=1.0, in1=xs[:], op0=ALU.add, op1=ALU.mult),
                 reads=[Retmp[j], Rsg[j]], writes=[Rsg[j]])
            yield
            S.op("act", I("activation", out=tt_[:], in_=tt_[:], func=AF.Exp, scale=-2.0 * C0), reads=[Rsg[j]], writes=[Rsg[j]])
            yield
            S.op("dve", I("tensor_scalar_add", out=tt_[:], in0=tt_[:], scalar1=1.0), reads=[Rsg[j]], writes=[Rsg[j]])
            yield
            S.op("dve", I("reciprocal", out=tt_[:], in_=tt_[:]), reads=[Rsg[j]], writes=[Rsg[j]])
            yield
            S.op("dve", I("tensor_tensor", out=out_ap, in0=xs[:], in1=tt_[:], op=ALU.mult), reads=[Retmp[j], Rsg[j]], writes=Rout)
            yield

        def gelu_gen2(j, pb, Rpb, out_ap, Rout, native=False):
            if native:
                S.op("act", I("activation", out=out_ap, in_=pb[:, :], func=AF.Gelu_apprx_tanh), reads=Rpb, writes=Rout)
                yield
                return
            xs, tt_ = etmp[j], sg_sb[j]
            S.op("act", I("activation", out=xs[:], in_=pb[:, :], func=AF.Copy), reads=Rpb, writes=[Retmp[j]])
            yield
            S.op("dve", I("scalar_tensor_tensor", out=tt_[:], in0=xs[:], scalar=C1, in1=xs[:], op0=ALU.mult, op1=ALU.mult),
                 reads=[Retmp[j]], writes=[Rsg[j]])
            yield
            S.op("dve", I("scalar_tensor_tensor", out=tt_[:], in0=tt_[:], scalar=1.0, in1=xs[:], op0=ALU.add, op1=ALU.mult),
                 reads=[Retmp[j], Rsg[j]], writes=[Rsg[j]])
            yield
            S.op("act", I("activation", out=tt_[:], in_=tt_[:], func=AF.Exp, scale=-2.0 * C0), reads=[Rsg[j]], writes=[Rsg[j]])
            yield
            S.op("dve", I("tensor_scalar_add", out=tt_[:], in0=tt_[:], scalar1=1.0), reads=[Rsg[j]], writes=[Rsg[j]])
            yield
            S.op("dve", I("reciprocal", out=tt_[:], in_=tt_[:]), reads=[Rsg[j]], writes=[Rsg[j]])
            yield
            S.op("dve", I("tensor_tensor", out=out_ap, in0=xs[:], in1=tt_[:], op=ALU.mult), reads=[Retmp[j], Rsg[j]], writes=Rout)
            yield

        def inproj_lanes(l, blk, native=False):
            w_in_v = w_in_d[l].rearrange("(k p) c -> p k c", p=128)
            qTb, RqTb = qT2[blk % 2], RqT2[blk % 2]
            g3 = lambda ap: ap.rearrange("p (g d) -> p g d", d=64)

            def norm_lane(tts):
                yield from rstd_batch_gen([blk * 4 + tt for tt in tts])
                for tt in tts:
                    yield from norm_gen(blk * 4 + tt, hT, [RhT], tt * 128, 0)

            LB = {0: (banks[0], [Rbank[0]]), 1: (banks[1], [Rbank[1]]), 2: (bank2f, [Rbank[2]])}

            def fm_group(kind, sh, wsl, Rw, cc, j, lb):
                c = sh * 2 + cc
                pb, Rpb = LB[lb]
                S.op("pe", [I("matmul", pb[:, :], wsl[:, k, cc * 128:(cc + 1) * 128], hT[:, k, :],
                              start=(k == 0), stop=(k == 7)) for k in range(8)], reads=[Rw, RhT], writes=Rpb)
                yield
                if kind == "q":
                    S.op("act", I("activation", out=qTb[:, c, :], in_=pb[:, :], func=AF.Copy, scale=0.125),
                         reads=Rpb, writes=[RqTb])
                    yield
                elif kind == "k":
                    S.op("dve", I("tensor_copy", out=kT[:, c, blk * 512:(blk + 1) * 512], in_=pb[:, :]),
                         reads=Rpb, writes=[Rbig[c * 4 + blk]])
                    yield
                else:
                    yield from gelu_gen2(j, pb, Rpb, uT[:, c, :], [RuT], native)

            def tm_group(kind, w0, Rw0, w1, Rw1, tt, j, lb):
                t = blk * 4 + tt
                pb, Rpb = LB[lb]
                fns = []
                for hf, wsl in ((0, w0), (1, w1)):
                    fns += [I("matmul", pb[:, hf * 256:(hf + 1) * 256], hT[:, k, tt * 128:(tt + 1) * 128], wsl[:, k, :],
                              start=(k == 0), stop=(k == 7)) for k in range(8)]
                S.op("pe", fns, reads=[Rw0, Rw1, RhT], writes=Rpb)
                yield
                if kind == "v":
                    S.op("dve", I("tensor_copy", out=v_sb[:, t, :], in_=pb[:, :]), reads=Rpb, writes=[Rbig[16 + t]])
                    yield
                else:
                    yield from gelu_gen2(j, pb, Rpb, gate_n[:, tt, :], [Rgn[tt]], native)
                    S.op("pool", I("tensor_tensor", out=sqb[j][:], in0=gate_n[:, tt, :], in1=gate_n[:, tt, :], op=ALU.mult),
                         reads=[Rgn[tt]], writes=[Rsqb[j]])
                    yield
                    S.op("dve", I("tensor_reduce", out=gss[:, tt * 8:(tt + 1) * 8], in_=g3(sqb[j][:]), axis=AX.X, op=ALU.add),
                         reads=[Rsqb[j]], writes=[Rgst[tt]])
                    yield

            def proj_lane_a():
                for kind, sbase in (("k", 2), ("q", 0)):
                    for sh in range(2):
                        wsl, Rw, hd = ws.next(w_in_v[:, :, (sbase + sh) * 256:(sbase + sh + 1) * 256], [128, 8, 256])
                        for cc in range(2):
                            yield from fm_group(kind, sh, wsl, Rw, cc, 0, 2)
                        ws.release(hd)
                w0, Rw0, h0 = ws.next(w_in_v[:, :, 4 * 256:5 * 256], [128, 8, 256])
                w1, Rw1, h1 = ws.next(w_in_v[:, :, 5 * 256:6 * 256], [128, 8, 256])
                for tt in range(4):
                    yield from tm_group("v", w0, Rw0, w1, Rw1, tt, 0, 2)
                ws.release(h0)
                ws.release(h1)

            def proj_lane_b():
                slc = []
                hds = []
                for sh in range(2):
                    a_, b_, c_ = ws.next(w_in_v[:, :, (6 + sh) * 256:(7 + sh) * 256], [128, 8, 256])
                    slc.append((a_, b_))
                    hds.append(c_)
                w0, Rw0, h0 = ws.next(w_in_v[:, :, 8 * 256:9 * 256], [128, 8, 256])
                w1, Rw1, h1 = ws.next(w_in_v[:, :, 9 * 256:10 * 256], [128, 8, 256])
                hds += [h0, h1]
                gens = []
                for sh in range(2):
                    for cc in range(2):
                        gens.append(("u", sh, cc))
                for tt in range(4):
                    gens.append(("g", tt, 0))
                subs = [[], []]
                for idx, it in enumerate(gens):
                    subs[idx % 2].append(it)

                def sub(j):
                    for it in subs[j]:
                        if it[0] == "u":
                            wsl, Rw = slc[it[1]]
                            yield from fm_group("u", it[1], wsl, Rw, it[2], j, j)
                        else:
                            yield from tm_group("g", w0, Rw0, w1, Rw1, it[1], j, j)
                a, b = sub(0), sub(1)
                alive = [a, b]
                while alive:
                    nxt = []
                    for g in alive:
                        if next(g, "END") != "END":
                            nxt.append(g)
                    alive = nxt
                    yield
                for hd in hds:
                    ws.release(hd)

            def gate_fin_lane(tts):
                for tt in tts:
                    S.op("act", I("activation", out=glv[:, tt * 8:(tt + 1) * 8], in_=gss[:, tt * 8:(tt + 1) * 8], func=AF.Ln, bias=EPS,
                                  scale=1.0 / 64), reads=[Rgst[tt]], writes=[Rgst[tt]])
                    yield
                    S.op("act", I("activation", out=grs[:, tt * 8:(tt + 1) * 8], in_=glv[:, tt * 8:(tt + 1) * 8], func=AF.Exp, scale=-0.5),
                         reads=[Rgst[tt]], writes=[Rgst[tt]])
                    yield
                    S.op("dve", I("tensor_tensor", out=g3(gate_n[:, tt, :]), in0=g3(gate_n[:, tt, :]),
                                  in1=grs[:, tt * 8:(tt + 1) * 8].unsqueeze(2).broadcast_to([128, 8, 64]), op=ALU.mult),
                         reads=[Rgn[tt], Rgst[tt]], writes=[Rgn[tt]])
                    yield
                    S.op("dve", I("tensor_tensor", out=gate_n[:, tt, :], in0=gate_n[:, tt, :], in1=gsgu_bc[:], op=ALU.mult),
                         reads=[Rgn[tt], Rgsgu], writes=[Rgn[tt]])
                    yield

            return [[norm_lane((0, 1, 2, 3))], [proj_lane_a(), proj_lane_b()], [gate_fin_lane((0, 2)), gate_fin_lane((1, 3))]]

        def sgu_lanes(l, blk):
            def lane():
                for tt in range(4):
                    bk = acc_bank()
                    S.op("pe", [I("matmul", banks[bk][(g % 2) * 64:(g % 2) * 64 + 64, (g // 2) * 128:(g // 2 + 1) * 128],
                                  gate_n[:, tt, g * 64:(g + 1) * 64], WsT[:, l, g, :], start=True, stop=True, skip_group_check=True)
                                for g in range(8)], reads=[Rgn[tt], RWs], writes=[Rbank[bk]])
                    yield
                    S.op("dve", I("tensor_tensor", out=stmp[:], in0=banks[bk][:, :], in1=B_sgu[:].rearrange("p j t -> p (j t)"),
                                  op=ALU.add), reads=[Rbank[bk], RB], writes=[Rstmp])
                    yield
                    S.op("dve", I("tensor_tensor", out=y_sb[:, 4:8, tt * 128:(tt + 1) * 128],
                                  in0=stmp[:].rearrange("p (j t) -> p j t", t=128), in1=uT[:, :, tt * 128:(tt + 1) * 128], op=ALU.mult),
                         reads=[Rstmp, RuT], writes=Ry[4:8])
                    yield
            return [[lane()]]

        def ynorm_outproj_lanes(l, blk):
            w_out_v = w_out_d[l].rearrange("(k p) c -> p k c", p=128)

            def ynorm_lane(i):
                for c in range(i, 8, 2):
                    S.op("pool", I("tensor_tensor", out=sqb[i][:], in0=y_sb[:, c, :], in1=y_sb[:, c, :], op=ALU.mult),
                         reads=[Ry[c]], writes=[Rsqb[i]])
                    yield
                    bk = i
                    S.op("pe", I("matmul", banks[bk][:, :], bones[:], sqb[i][:], start=True, stop=True),
                         reads=[Rsqb[i], Rconst], writes=[Rbank[bk]])
                    yield
                    S.op("act", I("activation", out=nl[i][:], in_=banks[bk][:, :], func=AF.Ln, bias=EPS, scale=1.0 / 64),
                         reads=[Rbank[bk]], writes=[Rnl[i]])
                    yield
                    S.op("act", I("activation", out=nl[i][:], in_=nl[i][:], func=AF.Exp, scale=-0.5), reads=[Rnl[i]], writes=[Rnl[i]])
                    yield
                    S.op("dve", I("scalar_tensor_tensor", out=y_sb[:, c, :], in0=y_sb[:, c, :], scalar=gout_c[:, c:c + 1],
                                  in1=nl[i][:], op0=ALU.mult, op1=ALU.mult), reads=[Ry[c], Rgout, Rnl[i]], writes=[Ry[c]])
                    yield

            def outproj_lane():
                for hf in range(2):
                    w0, Rw0, h0 = ws.next(w_out_v[:, :, (2 * hf) * 256:(2 * hf + 1) * 256], [128, 8, 256])
                    w1, Rw1, h1 = ws.next(w_out_v[:, :, (2 * hf + 1) * 256:(2 * hf + 2) * 256], [128, 8, 256])
                    for tt in range(4):
                        t = blk * 4 + tt
                        bk = acc_bank()
                        fns = []
                        for q_, wsl in ((0, w0), (1, w1)):
                            fns += [I("matmul", banks[bk][:, q_ * 256:(q_ + 1) * 256], y_sb[:, k, tt * 128:(tt + 1) * 128], wsl[:, k, :],
                                      start=(k == 0), stop=(k == 7)) for k in range(8)]
                        S.op("pe", fns, reads=[Rw0, Rw1] + Ry, writes=[Rbank[bk]])
                        yield
                        xs = x_sb[:, t, hf * 512:(hf + 1) * 512]
                        S.op("dve", I("tensor_tensor", out=xs, in0=xs, in1=banks[bk][:, :], op=ALU.add),
                             reads=[Rbank[bk], Rx[t]], writes=[Rx[t]])
                        yield
                    ws.release(h0)
                    ws.release(h1)
            return [[ynorm_lane(0), ynorm_lane(1)], [outproj_lane()]]

        def chain(*gens):
            for g in gens:
                yield from g

        def run(g):
            for _ in g:
                pass

        def phaseA(l, pre=None):
            load_gain(g_mix_d[l])
            S.dma("sp", I("dma_start", out=gsgu_bc[:], in_=g_sgu_d[l].partition_broadcast(128)), writes=[Rgsgu])
            for hh in range(2):
                S.dma("sp", I("dma_start", out=B_sgu[hh * 64:(hh + 1) * 64, :, :],
                              in_=sgu_b_d[l].rearrange("(j two) t -> two j t", two=2)[hh].partition_broadcast(64)), writes=[RB])
            S.dma("sp", I("dma_start", out=gout_c[:], in_=g_out_d[l].rearrange("(c p) -> p c", p=128),
                          allow_slow_non_contiguous=True), writes=[Rgout])
            if pre:
                for f in pre:
                    f()
            Lanes(inproj_lanes(l, 0, True) + sgu_lanes(l, 0), 1).tick(10 ** 6)
            for blk in range(4):
                first = None
                stages = []
                if blk > 0:
                    first = Lanes(ynorm_outproj_lanes(l, blk - 1), 36)
                    stages += sgu_lanes(l, blk)
                if blk < 3:
                    stages += inproj_lanes(l, blk + 1)
                side = Lanes(stages, (12 if blk > 0 else 0) + (85 if blk < 3 else 0)) if stages else None
                attention_block(blk, side, first)
            Lanes(ynorm_outproj_lanes(l, 3), 1).tick(10 ** 6)

        pending = []
        dacc = [0]
        DBANKS = (0, 1, 7, 2)

        def ffn_core(wg_v, wu_v, wd_rows, nch, gate_col):
            wgs, Rwg, hg = ws.next(wg_v, [128, 8, nch * 128])
            wus, Rwu, hu = ws.next(wu_v, [128, 8, nch * 128])
            wds, Rwd, hdn = ws.next(wd_rows.rearrange("(fc p) d -> p fc d", p=128), [128, nch, D])

            def gu_parts(tb, ai):
                hsegs = [Rbig[k * 4 + tb] for k in range(8)]
                parts = []
                for fc in range(nch):
                    st_ = {}

                    def g_part(fc=fc, st_=st_):
                        bg = 3 + (cnt["z"] % 2)
                        cnt["z"] += 1
                        st_["bg"] = bg
                        S.op("pe", [I("matmul", banks[bg][:, :], wgs[:, k, fc * 128:(fc + 1) * 128], h2T[:, k, tb * 512:(tb + 1) * 512],
                                      start=(k == 0), stop=(k == 7)) for k in range(8)], reads=[Rwg] + hsegs, writes=[Rbank[bg]])
                        S.op("act", I("activation", out=sg_sb[fc % 2][:], in_=banks[bg][:, :], func=AF.Silu),
                             reads=[Rbank[bg]], writes=[Rsg[fc % 2]])

                    def u_part(fc=fc, st_=st_):
                        bu = 5 + (cnt["c"] % 2)
                        cnt["c"] += 1
                        S.op("pe", [I("matmul", banks[bu][:, :], wus[:, k, fc * 128:(fc + 1) * 128], h2T[:, k, tb * 512:(tb + 1) * 512],
                                      start=(k == 0), stop=(k == 7)) for k in range(8)], reads=[Rwu] + hsegs, writes=[Rbank[bu]])
                        S.op("dve", I("tensor_tensor", out=aT[ai][:, fc, :], in0=sg_sb[fc % 2][:], in1=banks[bu][:, :], op=ALU.mult),
                             reads=[Rsg[fc % 2], Rbank[bu]], writes=[RaT[ai]])
                    parts += [g_part, u_part]
                return parts

            def down_parts(tb, ai):
                parts = []
                for tt in range(4):
                    for hf in range(2):
                        def d_part(tt=tt, hf=hf):
                            t = tb * 4 + tt
                            bk = DBANKS[dacc[0] % 4]
                            dacc[0] += 1
                            pbk = bank2f if bk == 2 else banks[bk]
                            S.op("pe", [I("matmul", pbk[:, :], aT[ai][:, fc, tt * 128:(tt + 1) * 128], wds[:, fc, hf * 512:(hf + 1) * 512],
                                          start=(fc == 0), stop=(fc == nch - 1)) for fc in range(nch)],
                                 reads=[Rwd, RaT[ai]], writes=[Rbank[bk]])
                            xs = x_sb[:, t, hf * 512:(hf + 1) * 512]
                            if gate_col is None:
                                S.op("dve", I("tensor_tensor", out=xs, in0=xs, in1=pbk[:, :], op=ALU.add),
                                     reads=[Rbank[bk], Rx[t]], writes=[Rx[t]])
                            else:
                                S.op("dve", I("scalar_tensor_tensor", out=xs, in0=pbk[:, :], scalar=gates[:, t, gate_col:gate_col + 1],
                                              in1=xs, op0=ALU.mult, op1=ALU.add), reads=[Rbank[bk], Rx[t], Rgates[t]], writes=[Rx[t]])
                        parts.append(d_part)
                return parts

            for tb in range(4):
                ai = cnt["u"] % 2
                cnt["u"] += 1
                gp = gu_parts(tb, ai)
                dp = list(pending)
                del pending[:]
                per = (len(dp) + len(gp) - 1) // len(gp)
                for g in gp:
                    g()
                    for _ in range(per):
                        if dp:
                            dp.pop(0)()
                while dp:
                    dp.pop(0)()
                pending.extend(down_parts(tb, ai))
            ws.release(hg)
            ws.release(hu)
            pending.append(lambda: ws.release(hdn))

        def ffn_flush():
            while pending:
                pending.pop(0)()

        def router_tile(t):
            bk = acc_bank()
            S.op("pe", [I("matmul", banks[bk][:, 0:NE], h2T[:, k, t * 128:(t + 1) * 128], router_sb[:, k, :],
                          start=(k == 0), stop=(k == 7)) for k in range(8)],
                 reads=[Rrouter] + [Rbig[k * 4 + t // 4] for k in range(8)], writes=[Rbank[bk]])
            L = dict(reads=[Rlog], writes=[Rlog])
            S.op("dve", I("tensor_copy", out=logit[:], in_=banks[bk][:, 0:NE]), reads=[Rbank[bk]], writes=[Rlog])
            S.op("dve", I("tensor_reduce", out=m1[:, 0:1], in_=logit[:], axis=AX.X, op=ALU.max), **L)
            S.op("dve", I("tensor_scalar", out=mk1[:], in0=logit[:], scalar1=m1[:, 0:1], scalar2=None, op0=ALU.is_equal), **L)
            S.op("dve", I("scalar_tensor_tensor", out=l2[:], in0=mk1[:], scalar=-1e30, in1=logit[:], op0=ALU.mult, op1=ALU.add), **L)
            S.op("dve", I("tensor_reduce", out=m1[:, 1:2], in_=l2[:], axis=AX.X, op=ALU.max), **L)
            S.op("dve", I("tensor_scalar", out=mk2[:], in0=l2[:], scalar1=m1[:, 1:2], scalar2=None, op0=ALU.is_equal), **L)
            S.op("dve", I("tensor_tensor", out=m1[:, 2:3], in0=m1[:, 1:2], in1=m1[:, 0:1], op=ALU.subtract), **L)
            S.op("act", I("activation", out=m1[:, 2:3], in_=m1[:, 2:3], func=AF.Exp), **L)
            S.op("dve", I("tensor_scalar_add", out=m1[:, 3:4], in0=m1[:, 2:3], scalar1=1.0), **L)
            S.op("dve", I("reciprocal", out=m1[:, 3:4], in_=m1[:, 3:4]), **L)
            S.op("dve", I("tensor_tensor", out=m1[:, 2:3], in0=m1[:, 2:3], in1=m1[:, 3:4], op=ALU.mult), **L)
            S.op("dve", I("tensor_scalar", out=mk1[:], in0=mk1[:], scalar1=m1[:, 3:4], scalar2=None, op0=ALU.mult), **L)
            S.op("dve", I("scalar_tensor_tensor", out=gates[:, t, :], in0=mk2[:], scalar=m1[:, 2:3], in1=mk1[:],
                          op0=ALU.mult, op1=ALU.add), reads=[Rlog], writes=[Rgates[t]])

        def phaseB(l):
            load_gain(g_ffn_d[l])
            for q4 in range(4):
                run(rstd_batch_gen(list(range(q4 * 4, q4 * 4 + 4))))
            for t in range(NT):
                norm_tile(t, h2T, [Rbig[k * 4 + t // 4] for k in range(8)], t * 128)
            i = l // 2
            if l % 2 == 0:
                wg_v = ffn_wg_d[i].rearrange("(k p) c -> p k c", p=128)
                wu_v = ffn_wu_d[i].rearrange("(k p) c -> p k c", p=128)
                for fg in range(D_FF // 256):
                    ffn_core(wg_v[:, :, fg * 256:(fg + 1) * 256], wu_v[:, :, fg * 256:(fg + 1) * 256],
                             ffn_wd_d[i][fg * 256:(fg + 1) * 256, :], 2, None)
                ffn_flush()
            else:
                for t in range(NT):
                    router_tile(t)
                for ex in range(NE):
                    wg_v = moe_wg_d[i, ex].rearrange("(k p) c -> p k c", p=128)
                    wu_v = moe_wu_d[i, ex].rearrange("(k p) c -> p k c", p=128)
                    f0 = 0
                    while f0 < D_FFE:
                        nch = min(2, (D_FFE - f0) // 128)
                        ffn_core(wg_v[:, :, f0:f0 + nch * 128], wu_v[:, :, f0:f0 + nch * 128],
                                 moe_wd_d[i, ex][f0:f0 + nch * 128, :], nch, ex)
                        f0 += nch * 128
                ffn_flush()

        fin = []

        def program():
            setup_consts()
            for s in range(nseq):
                for t in range(4):
                    S.dma("sp", I("dma_start", out=x_sb[:, t, :], in_=x_d[s, t * 128:(t + 1) * 128, :]), writes=[Rx[t]])
                rest = [(lambda t=t, s=s: S.dma("sp", I("dma_start", out=x_sb[:, t, :], in_=x_d[s, t * 128:(t + 1) * 128, :]),
                                               writes=[Rx[t]])) for t in range(4, NT)]
                done = False
                for l in range(depth):
                    phaseA(l, rest if l == 0 else None)
                    if stop == "A%d" % l:
                        done = True
                        break
                    phaseB(l)
                    if stop == "F%d" % l:
                        done = True
                        break
                if not done:
                    load_gain(g_final_d)
                    for q4 in range(4):
                        run(rstd_batch_gen(list(range(q4 * 4, q4 * 4 + 4))))
                    for t in range(NT):
                        S.op("dve", I("scalar_tensor_tensor", out=x_sb[:, t, :], in0=x_sb[:, t, :], scalar=rstd[:, t:t + 1],
                                      in1=gain_bc[:], op0=ALU.mult, op1=ALU.mult), reads=[Rx[t], Rss[t], Rgain], writes=[Rx[t]])
                for t in range(NT):
                    stp = S.dma("sp", I("dma_start", out=out_d[s, t * 128:(t + 1) * 128, :], in_=x_sb[:, t, :]), reads=[Rx[t]])
                    if stp is not None:
                        fin.append(stp)

        S.dry = True
        ws.record = True
        program()
        S.dry = False
        ws.start_real()
        for k in cnt:
            cnt[k] = 0
        acc_rr[0] = 0
        dacc[0] = 0
        program()
        S.final_wait("sp", fin)
        print("sbuf remaining", nc.sbuf_bytes_remaining, "ops", S.n_ins, "waits", S.n_wait, "wslices", len(ws.plan))
        with nc.Block() as block:
            S.emit(block)
    return nc


_WEIGHT_NAMES = ["w_in", "w_out", "g_mix", "g_ffn", "g_sgu", "sgu_w", "sgu_b", "g_out", "ffn_w_gate", "ffn_w_up",
                 "ffn_w_down", "router_w", "moe_w_gate", "moe_w_up", "moe_w_down", "g_final"]


def kernel(**inputs):
    x = np.ascontiguousarray(np.asarray(inputs["x"], dtype=np.float32))
    wts = {n: np.ascontiguousarray(np.asarray(inputs[n], dtype=np.float32)) for n in _WEIGHT_NAMES}
    nc = build()
    in_maps = []
    for c in range(NCORES):
        m = {"x": x[c * SEQ_PER_CORE:(c + 1) * SEQ_PER_CORE]}
        m.update(wts)
        in_maps.append(m)
    res = run_bass_kernel_spmd(nc, in_maps, core_ids=list(range(NCORES)))
    out = np.concatenate([np.asarray(r["out"]) for r in res.results], axis=0)
    return out.astype(np.float32, copy=False)
```

```python
import contextlib
from collections import deque

import numpy as np
import concourse.bass as bass
import concourse.mybir as mybir
from concourse.bass_utils import run_bass_kernel_spmd

F32 = mybir.dt.float32
BF16 = mybir.dt.bfloat16
AF = mybir.ActivationFunctionType
ALU = mybir.AluOpType
AX = mybir.AxisListType

D = 1024
SEQ = 2048
NT = SEQ // 128
DEPTH = 2
D_IN = 2560
D_FF = 2816
NE = 8
D_FFE = 1408
EPS = 1e-6
NCORES = 8
SEQ_PER_CORE = 4
NSLOT = 8


def I(name, *a, **kw):
    return lambda e: getattr(e, name)(*a, **kw)


class R:
    __slots__ = ("w", "r")

    def __init__(self):
        self.w = None
        self.r = {}


class Sched:
    ENGS = ("pe", "act", "dve", "pool", "sp")

    def __init__(self, nc, stack, n_dma_sems=32, n_fresh=0):
        self.nc = nc
        self.dry = False
        self.fresh = [stack.enter_context(nc.semaphore("s_fr%d" % i)) for i in range(n_fresh)]
        self.fresh_i = 0
        self.q = {e: [] for e in self.ENGS}
        self.sems = {}
        self.count = {}
        for e in self.ENGS:
            self.sems[e] = stack.enter_context(nc.semaphore("s_" + e))
            self.count[e] = 0
        self.n_dma = n_dma_sems
        for i in range(n_dma_sems):
            k = ("dma", i)
            self.sems[k] = stack.enter_context(nc.semaphore("s_dma%d" % i))
            self.count[k] = 0
        self.dma_rr = 0
        self.n_pdma = 16
        for i in range(self.n_pdma):
            k = ("pdma", i)
            self.sems[k] = stack.enter_context(nc.semaphore("s_pdma%d" % i))
            self.count[k] = 0
        self.pdma_rr = 0
        self.seen = {e: {} for e in self.ENGS}
        self.n_wait = 0
        self.n_ins = 0

    def _waits(self, eng, reads, writes, extra=()):
        need = {}

        def add(st):
            if st is None:
                return
            k, v = st
            if need.get(k, 0) < v:
                need[k] = v

        for r in reads:
            add(r.w)
        for w in writes:
            add(w.w)
            for k, v in w.r.items():
                add((k, v))
        for st in extra:
            add(st)
        out = []
        seen = self.seen[eng]
        for k, v in need.items():
            if seen.get(k, 0) >= v:
                continue
            seen[k] = v
            out.append((self.sems[k], v))
        return out

    def _mark(self, stamp, reads, writes):
        k, v = stamp
        for w in writes:
            w.w = stamp
            w.r = {}
        for r in reads:
            if r.r.get(k, 0) < v:
                r.r[k] = v

    def op(self, eng, fns, reads=(), writes=()):
        if self.dry:
            return None
        if callable(fns):
            fns = [fns]
        waits = self._waits(eng, reads, writes)
        self.count[eng] += 1
        stamp = (eng, self.count[eng])
        sem = self.sems[eng]
        self.n_wait += len(waits)
        self.n_ins += len(fns)

        def thunk(e, waits=waits, fns=fns, sem=sem):
            for s, v in waits:
                e.wait_ge(s, v)
            last = None
            for f in fns:
                last = f(e)
            last.then_inc(sem, 1)

        self.q[eng].append(thunk)
        self._mark(stamp, reads, writes)
        return stamp

    def dma(self, eng, fn, reads=(), writes=()):
        if self.dry:
            return None
        if eng == "pool" and self.fresh:
            k = ("fresh", self.fresh_i)
            self.sems[k] = self.fresh[self.fresh_i]
            self.count[k] = 0
            self.fresh_i += 1
        elif eng == "pool":
            i = self.pdma_rr
            self.pdma_rr = (self.pdma_rr + 1) % self.n_pdma
            k = ("pdma", i)
        else:
            i = self.dma_rr
            self.dma_rr = (self.dma_rr + 1) % self.n_dma
            k = ("dma", i)
        prev = (k, self.count[k]) if self.count[k] else None
        waits = self._waits(eng, reads, writes, extra=(prev,) if prev else ())
        self.count[k] += 16
        stamp = (k, self.count[k])
        sem = self.sems[k]
        self.n_wait += len(waits)
        self.n_ins += 1

        def thunk(e, waits=waits, fn=fn, sem=sem):
            for s, v in waits:
                e.wait_ge(s, v)
            fn(e).then_inc(sem, 16)

        self.q[eng].append(thunk)
        self._mark(stamp, reads, writes)
        return stamp

    def final_wait(self, eng, stamps):
        need = {}
        for k, v in stamps:
            if need.get(k, 0) < v:
                need[k] = v
        ws = [(self.sems[k], v) for k, v in need.items()]

        def thunk(e, ws=ws):
            for s, v in ws:
                e.wait_ge(s, v)

        self.q[eng].append(thunk)

    def emit(self, block):
        q = self.q

        @block.tensor
        def _(e):
            for t in q["pe"]:
                t(e)

        @block.scalar
        def _(e):
            for t in q["act"]:
                t(e)

        @block.vector
        def _(e):
            for t in q["dve"]:
                t(e)

        @block.gpsimd
        def _(e):
            for t in q["pool"]:
                t(e)

        @block.sync
        def _(e):
            for t in q["sp"]:
                t(e)


class Lanes:
    def __init__(self, stages, est_ticks):
        self.stages = [list(st) for st in stages if st]
        self.est = max(1, est_ticks)
        self.used = 0

    def done(self):
        return not self.stages

    def tick(self, k=1):
        for _ in range(k):
            if not self.stages:
                return
            self.used += 1
            lanes = self.stages[0]
            alive = []
            for g in lanes:
                if next(g, "END") != "END":
                    alive.append(g)
            if alive:
                self.stages[0] = alive
            else:
                self.stages.pop(0)

    def ticks_needed(self, steps_left):
        rem = max(1, self.est - self.used)
        return max(1, (rem + max(1, steps_left) - 1) // max(1, steps_left))


class WStream:
    def __init__(self, S, slots, nslot):
        self.S = S
        self.slots = slots
        self.R = [R() for _ in range(nslot)]
        self.nslot = nslot
        self.plan = []
        self.record = True
        self.i = 0
        self.issued = 0
        self.free = list(range(nslot))
        self.slot_of = {}

    def start_real(self):
        self.record = False
        self.i = 0
        self.issued = 0
        self.free = list(range(self.nslot))
        self.slot_of = {}

    def _view(self, k, shape):
        n = 1
        for s_ in shape[1:]:
            n *= s_
        v = self.slots[k][:, 0:n]
        if len(shape) == 3:
            v = v.rearrange("p (a b) -> p a b", b=shape[2])
        return v

    def _prefetch(self):
        while self.issued < len(self.plan) and self.free and self.issued - self.i < 4:
            j = self.issued
            k = self.free.pop(0)
            src, shape = self.plan[j]
            dst = self._view(k, shape)
            self.S.dma("pool", lambda e, dst=dst, src=src: e.dma_start(out=dst, in_=src), writes=[self.R[k]])
            self.slot_of[j] = k
            self.issued += 1

    def next(self, src, shape):
        i = self.i
        self.i += 1
        if self.record:
            self.plan.append((src, shape))
            return self._view(0, shape), self.R[0], None
        self._prefetch()
        assert i in self.slot_of, "weight ring exhausted: too many live slices"
        k = self.slot_of[i]
        return self._view(k, shape), self.R[k], (i, k)

    def release(self, handle):
        if handle is None:
            return
        i, k = handle
        self.free.append(k)
        self._prefetch()


def build(nseq=SEQ_PER_CORE, depth=DEPTH, stop=None, n_fresh=0, ndummy=0):
    nc = bass.Bass("TRN2", target_bir_lowering=False)
    dt = lambda name, shape: nc.dram_tensor(name, shape, F32, kind="ExternalInput").ap()
    x_d = dt("x", [nseq, SEQ, D])
    w_in_d = dt("w_in", [DEPTH, D, D_IN])
    w_out_d = dt("w_out", [DEPTH, D, D])
    g_mix_d = dt("g_mix", [DEPTH, D])
    g_ffn_d = dt("g_ffn", [DEPTH, D])
    g_sgu_d = dt("g_sgu", [DEPTH, 512])
    sgu_w_d = dt("sgu_w", [DEPTH, 8, 128, 128])
    sgu_b_d = dt("sgu_b", [DEPTH, 8, 128])
    g_out_d = dt("g_out", [DEPTH, D])
    ffn_wg_d = dt("ffn_w_gate", [1, D, D_FF])
    ffn_wu_d = dt("ffn_w_up", [1, D, D_FF])
    ffn_wd_d = dt("ffn_w_down", [1, D_FF, D])
    router_d = dt("router_w", [1, D, NE])
    moe_wg_d = dt("moe_w_gate", [1, NE, D, D_FFE])
    moe_wu_d = dt("moe_w_up", [1, NE, D, D_FFE])
    moe_wd_d = dt("moe_w_down", [1, NE, D_FFE, D])
    g_final_d = dt("g_final", [D])
    out_d = nc.dram_tensor("out", [nseq, SEQ, D], F32, kind="ExternalOutput").ap()

    with contextlib.ExitStack() as st:
        T = lambda name, shape, dtype=F32: st.enter_context(nc.sbuf_tensor(name, shape, dtype))
        x_sb = T("x_sb", [128, NT, D])
        big = T("big", [128, 16384], BF16)
        kT = big[:, 0:8192].rearrange("p (c t) -> p c t", t=SEQ)
        v_sb = big[:, 8192:16384].rearrange("p (t d) -> p t d", d=512)
        h2T = big[:, :].rearrange("p (k t) -> p k t", t=SEQ)
        Rbig = [R() for _ in range(32)]
        hT = T("hT", [128, 8, 512], BF16)
        qT2 = [T("qT%d" % i, [128, 4, 512], BF16) for i in range(2)]
        qT = qT2[0]
        uT = T("uT", [128, 4, 512], BF16)
        gate_n = T("gate_n", [128, 4, 512], BF16)
        y_sb = T("y_sb", [128, 8, 512], BF16)
        slots = [T("wslot%d" % i, [128, 2048], BF16) for i in range(NSLOT)]
        E_sb = [T("E%d" % i, [128, 1024], BF16) for i in range(3)]
        P_sb = [T("P%d" % i, [128, 1024], BF16) for i in range(2)]
        G_sb = [T("G%d" % i, [128, 1024], BF16) for i in range(2)]
        A_sb = [T("A0", [128, 1024], BF16)]
        PS_sb = T("PS", [128, 1024], BF16)
        xn0 = T("xn0", [128, D], BF16)
        xn = [xn0, xn0]
        junk = T("junk", [128, D], BF16)
        ss = T("ss", [128, NT])
        lnv = T("lnv", [128, NT])
        rstd = T("rstd", [128, NT])
        gss = T("gss", [128, 32])
        glv = T("glv", [128, 32])
        grs = T("grs", [128, 32])
        sqb = [T("sqb%d" % i, [128, 512], BF16) for i in range(2)]
        sg_sb = [T("sg%d" % i, [128, 512]) for i in range(2)]
        aT = [qT[:, 0:2, :], uT[:, 0:2, :]]
        etmp = [T("etmp%d" % i, [128, 512]) for i in range(2)]
        stmp = etmp[0]
        nl = [etmp[1], sg_sb[1]]
        ident = T("ident", [128, 128], BF16)
        tri = T("tri", [128, 128], BF16)
        ones = T("ones", [128, 128], BF16)
        bones = T("bones", [128, 128], BF16)
        mask = T("mask", [128, 128], BF16)
        cf = T("cf", [128, 128])
        gain_bc = T("gain_bc", [128, D])
        gsgu_bc = T("gsgu_bc", [128, 512])
        B_sgu = T("B_sgu", [128, 4, 128])
        gout_c = T("gout_c", [128, 8])
        WsT = T("WsT", [128, DEPTH, 8, 128], BF16)
        wtmp = gain_bc[:, :].rearrange("p (g t) -> p g t", t=128)
        wtmpb = junk[:, :].rearrange("p (g t) -> p g t", t=128)
        router_sb = T("router_sb", [128, 8, NE], BF16)
        logit = T("logit", [128, NE])
        m1 = T("m1", [128, 4])
        mk1 = T("mk1", [128, NE])
        mk2 = T("mk2", [128, NE])
        l2 = T("l2", [128, NE])
        gates = T("gates", [128, NT, NE])

        banks = []
        for i in range(3):
            if i == 2:
                banks.append(st.enter_context(nc.psum_tensor("bank2", [128, 1024], BF16)))
            else:
                banks.append(st.enter_context(nc.psum_tensor("bank%d" % i, [128, 512], F32)))
        zz = st.enter_context(nc.psum_tensor("zz", [128, 1024], F32))
        cc = st.enter_context(nc.psum_tensor("cc", [128, 1024], F32))
        banks += [zz[:, 0:512], zz[:, 512:1024], cc[:, 0:512], cc[:, 512:1024]]
        banks.append(st.enter_context(nc.psum_tensor("bank7", [128, 512], F32)))
        pst = banks[2]
        bank2f = banks[2][:, :].bitcast(F32)
        Rbank = [R() for _ in range(8)]
        Rpst = [Rbank[2], Rbank[2]]
        S = Sched(nc, st, n_dma_sems=(8 if n_fresh else 24), n_fresh=n_fresh)
        ws = WStream(S, slots, NSLOT)

        Rx = [R() for _ in range(NT)]
        RhT = R(); RqT2 = [R(), R()]; RqT = RqT2[0]; RuT = R(); Rgn = [R() for _ in range(4)]
        Ry = [R() for _ in range(8)]
        RE = [R(), R(), R()]; RP = [R(), R()]; RG = [R(), R()]; RA = [R()]; RPS = [R(), R()]
        Rxn0 = R(); Rxn = [Rxn0, Rxn0]; Rjunk = R(); Rss = [R() for _ in range(NT)]
        Rgst = [R() for _ in range(4)]
        Rsqb = [R(), R()]; Rsg = [R(), R()]; RaT = [RqT, RuT]; Retmp = [R(), R()]; Rstmp = Retmp[0]; Rnl = [Retmp[1], Rsg[1]]
        Rconst = R(); Rgain = R(); Rgsgu = R(); RB = R(); Rgout = R(); RWs = R(); Rwtmp = Rgain; Rwtmpb = Rjunk; Rcf = R()
        Rrouter = R(); Rlog = R(); Rgates = [R() for _ in range(NT)]

        acc_rr = [0]

        def acc_bank():
            b = acc_rr[0]
            acc_rr[0] = (b + 1) % 2
            return b

        cnt = {"z": 0, "c": 0, "u": 0, "ev": 0, "et": 0}

        def setup_consts():
            def build_mask(dst, pattern, cmp_op, cm):
                S.op("pool", I("memset", cf[:], 1.0), writes=[Rcf])
                S.op("pool", I("affine_select", out=cf[:], in_=cf[:], pattern=pattern, compare_op=cmp_op,
                               fill=0.0, base=0, channel_multiplier=cm), reads=[Rcf], writes=[Rcf])
                S.op("dve", I("tensor_copy", out=dst[:], in_=cf[:]), reads=[Rcf], writes=[Rconst])
            build_mask(ident, [[-1, 128]], ALU.is_equal, 1)
            build_mask(tri, [[-1, 128]], ALU.is_ge, 1)
            S.op("pool", I("memset", cf[:], -30000.0), writes=[Rcf])
            S.op("pool", I("affine_select", out=cf[:], in_=cf[:], pattern=[[-1, 128]], compare_op=ALU.is_ge,
                           fill=0.0, base=0, channel_multiplier=1), reads=[Rcf], writes=[Rcf])
            S.op("dve", I("tensor_copy", out=mask[:], in_=cf[:]), reads=[Rcf], writes=[Rconst])
            S.op("dve", [I("memset", ones[:], 1.0), I("memset", bones[:], 0.0)], writes=[Rconst])
            S.op("dve", [I("memset", bones[0:64, 0:64], 1.0), I("memset", bones[64:128, 64:128], 1.0)], writes=[Rconst])
            for l in range(depth):
                S.dma("sp", I("dma_start", out=wtmp[:], in_=sgu_w_d[l].rearrange("g t s -> t g s")), writes=[Rwtmp])
                S.op("dve", I("tensor_copy", out=wtmpb[:], in_=wtmp[:]), reads=[Rwtmp], writes=[Rwtmpb])
                S.op("pe", [I("transpose", out=pst[:, g * 128:(g + 1) * 128], in_=wtmpb[:, g, :], identity=ident[:])
                            for g in range(8)], reads=[Rwtmpb, Rconst], writes=[Rbank[2]])
                S.op("dve", I("tensor_copy", out=wtmpb[:].rearrange("p g t -> p (g t)"), in_=pst[:, 0:1024]),
                     reads=[Rbank[2]], writes=[Rwtmpb])
                S.op("pool", [I("affine_select", out=WsT[:, l, g, :], in_=wtmpb[:, g, :], pattern=[[1, 128]],
                                compare_op=ALU.is_ge, fill=0.0, base=0, channel_multiplier=-1) for g in range(8)],
                     reads=[Rwtmpb], writes=[RWs])
            if depth > 1:
                S.dma("pool", I("dma_start", out=router_sb[:], in_=router_d[0].rearrange("(k p) e -> p k e", p=128)),
                      writes=[Rrouter])

        def rstd_batch_gen(tiles):
            t0, t1 = tiles[0], tiles[-1] + 1
            Rs = [Rss[t] for t in tiles]
            S.op("pool", I("memset", ss[:, t0:t1], 0.0), writes=Rs)
            yield
            for t in tiles:
                S.op("act", I("activation", out=junk[:], in_=x_sb[:, t, :], func=AF.Square, accum_out=ss[:, t:t + 1]),
                     reads=[Rx[t]], writes=[Rjunk, Rss[t]])
                yield
            S.op("act", I("activation", out=lnv[:, t0:t1], in_=ss[:, t0:t1], func=AF.Ln, bias=EPS, scale=1.0 / D), reads=Rs, writes=Rs)
            yield
            S.op("act", I("activation", out=rstd[:, t0:t1], in_=lnv[:, t0:t1], func=AF.Exp, scale=-0.5), reads=Rs, writes=Rs)
            yield

        def norm_gen(t, dest, Rdest, col0, i=None):
            if i is None:
                i = cnt["u"] % 2
                cnt["u"] += 1
            S.op("dve", I("scalar_tensor_tensor", out=xn[i][:], in0=x_sb[:, t, :], scalar=rstd[:, t:t + 1], in1=gain_bc[:],
                          op0=ALU.mult, op1=ALU.mult), reads=[Rx[t], Rss[t], Rgain], writes=[Rxn[i]])
            yield
            ph = pst[:, i * 512:(i + 1) * 512]
            for half in range(2):
                S.op("pe", [I("transpose", out=ph[:, k * 128:(k + 1) * 128], in_=xn[i][:, (half * 4 + k) * 128:(half * 4 + k + 1) * 128],
                              identity=ident[:]) for k in range(4)], reads=[Rxn[i], Rconst], writes=[Rpst[i]])
                yield
                src = ph.rearrange("p (k t) -> p k t", t=128)
                dst = dest[:, half * 4:(half + 1) * 4, col0:col0 + 128]
                if cnt["ev"] % 2 == 0:
                    S.op("dve", I("tensor_copy", out=dst, in_=src), reads=[Rpst[i]], writes=Rdest)
                else:
                    S.op("act", I("activation", out=dst, in_=src, func=AF.Copy), reads=[Rpst[i]], writes=Rdest)
                cnt["ev"] += 1
                yield

        def norm_tile(t, dest, Rdest, col0):
            for _ in norm_gen(t, dest, Rdest, col0):
                pass

        def load_gain(g_ap):
            S.dma("sp", I("dma_start", out=gain_bc[:], in_=g_ap.partition_broadcast(128)), writes=[Rgain])

        def attention_block(blk, side=None, first=None):
            bo = 7
            qT = qT2[blk % 2]
            RqT = RqT2[blk % 2]
            Rzz = [Rbank[3], Rbank[4]]
            Rcc = [Rbank[5], Rbank[6]]
            units = [(c, kb) for c in range(4) for kb in range(4 * blk + 3, -1, -1)]
            n = len(units)
            V3 = lambda ap: ap.rearrange("p (h q) -> p h q", h=2)

            def prm(i):
                c, kb = units[i]
                col0 = max(0, kb * 128 - blk * 512)
                return c, kb, col0, kb >= 4 * blk, kb == 4 * blk + 3

            def stA1(i):
                c, kb, col0, diag, first_ = prm(i)
                E = E_sb[i % 3]
                fz = []
                for hi in range(2):
                    hb = hi * 64
                    fz.append(I("matmul", zz[:, hi * 512 + col0:hi * 512 + 512], kT[hb:hb + 64, c, kb * 128:(kb + 1) * 128],
                                qT[hb:hb + 64, c, col0:512], start=True, stop=not diag, skip_group_check=True))
                if diag:
                    for hi in range(2):
                        fz.append(I("matmul", zz[:, hi * 512 + col0:hi * 512 + col0 + 128], ident[:], mask[:], start=False, stop=True,
                                    skip_group_check=True))
                S.op("pe", fz, reads=[Rbig[c * 4 + kb // 4], RqT, Rconst], writes=Rzz)
                S.op("act", I("activation", out=V3(E[:, :])[:, :, col0:512], in_=V3(zz[:, :])[:, :, col0:512], func=AF.Exp),
                     reads=Rzz, writes=[RE[i % 3]])

            def stA2(i):
                c, kb, col0, diag, first_ = prm(i)
                E, P = E_sb[i % 3], P_sb[i % 2]
                S.op("act", I("activation", out=V3(P[:, :])[:, :, col0:512], in_=V3(E[:, :])[:, :, col0:512], func=AF.Ln, bias=1.0),
                     reads=[RE[i % 3]], writes=[RP[i % 2]])

            def stB(i):
                c, kb, col0, diag, first_ = prm(i)
                P, G, PS = P_sb[i % 2], G_sb[i % 2], PS_sb
                for hi in range(2):
                    o = hi * 512
                    fns = [I("matmul", cc[:, o + col0:o + 512], tri[:], P[:, o + col0:o + 512], start=True, stop=first_,
                             skip_group_check=True)]
                    if not first_:
                        fns.append(I("matmul", cc[:, o + col0:o + 512], ones[:], PS[:, o + col0:o + 512], start=False, stop=True,
                                     skip_group_check=True))
                    S.op("pe", fns, reads=[RP[i % 2], Rconst] + ([] if first_ else [RPS[hi]]), writes=[Rcc[hi]])
                    if kb > 0:
                        if first_:
                            fl = [I("tensor_copy", out=PS[:, o + col0:o + 512], in_=P[:, o + col0:o + 512])]
                            if col0 > 0:
                                fl.append(I("memset", PS[:, o:o + col0], 0.0))
                            S.op("pool", fl, reads=[RP[i % 2]], writes=[RPS[hi]])
                        else:
                            S.op("pool", I("tensor_tensor", out=PS[:, o + col0:o + 512], in0=PS[:, o + col0:o + 512],
                                           in1=P[:, o + col0:o + 512], op=ALU.add), reads=[RP[i % 2], RPS[hi]], writes=[RPS[hi]])
                S.op("act", I("activation", out=V3(G[:, :])[:, :, col0:512], in_=V3(cc[:, :])[:, :, col0:512], func=AF.Exp, scale=-1.0),
                     reads=Rcc, writes=[RG[i % 2]])

            def stC(i):
                c, kb, col0, diag, first_ = prm(i)
                E, G, A = E_sb[i % 3], G_sb[i % 2], A_sb[0]
                S.op("dve", I("tensor_tensor", out=V3(A[:, :])[:, :, col0:512], in0=V3(E[:, :])[:, :, col0:512],
                              in1=V3(G[:, :])[:, :, col0:512], op=ALU.mult), reads=[RE[i % 3], RG[i % 2]], writes=[RA[0]])
                fns = []
                for hi in range(2):
                    hb = hi * 64
                    h = 2 * c + hi
                    fns.append(I("matmul", banks[bo][hb:hb + 64, col0:512], v_sb[:, kb, h * 64:(h + 1) * 64],
                                 A[:, hi * 512 + col0:hi * 512 + 512], start=first_, stop=(kb == 0), skip_group_check=True))
                S.op("pe", fns, reads=[RA[0], Rbig[16 + kb]], writes=[Rbank[bo]])
                if kb == 0:
                    S.op("act", I("activation", out=y_sb[:, c, :], in_=banks[bo][:, :], func=AF.Copy), reads=[Rbank[bo]], writes=[Ry[c]])

            nf_steps = max(1, n // 4 - 3)
            for i in range(n + 2):
                if i < n:
                    stA1(i)
                    stA2(i)
                if 0 <= i - 1 < n:
                    stB(i - 1)
                if 0 <= i - 2 < n:
                    stC(i - 2)
                if first is not None:
                    k = first.ticks_needed(nf_steps - i) if i < nf_steps else 10 ** 6
                    first.tick(k)
                    if first.done():
                        first = None
                elif side is not None:
                    side.tick(side.ticks_needed(n - i))
                    if side.done():
                        side = None
            if first is not None:
                first.tick(10 ** 6)
            if side is not None:
                side.tick(10 ** 6)

        C0 = 0.7978845608028654
        C1 = 0.044715

        def gelu_gen(bk, out_ap, Rout):
            j = 0
            xs, tt_ = etmp[j], sg_sb[j]
            S.op("act", I("activation", out=xs[:], in_=banks[bk][:, :], func=AF.Copy), reads=[Rbank[bk]], writes=[Retmp[j]])
            yield
            S.op("dve", I("scalar_tensor_tensor", out=tt_[:], in0=xs[:], scalar=C1, in1=xs[:], op0=ALU.mult, op1=ALU.mult),
                 reads=[Retmp[j]], writes=[Rsg[j]])
            yield
            S.op("dve", I("scalar_tensor_tensor", out=tt_[:], in0=tt_[:], scalar=1.0, in1=xs[:], op0=ALU.add, op1=ALU.mult),
                 reads=[Retmp[j], Rsg[j]], writes=[Rsg[j]])
            yield
            S.op("act", I("activation", out=tt_[:], in_=tt_[:], func=AF.Exp, scale=-2.0 * C0), reads=[Rsg[j]], writes=[Rsg[j]])
            yield
            S.op("dve", I("tensor_scalar_add", out=tt_[:], in0=tt_[:], scalar1=1.0), reads=[Rsg[j]], writes=[Rsg[j]])
            yield
            S.op("dve", I("reciprocal", out=tt_[:], in_=tt_[:]), reads=[Rsg[j]], writes=[Rsg[j]])
            yield
            S.op("dve", I("tensor_tensor", out=out_ap, in0=xs[:], in1=tt_[:], op=ALU.mult), reads=[Retmp[j], Rsg[j]], writes=Rout)
            yield

        def gelu_gen2(j, pb, Rpb, out_ap, Rout, native=False):
            if native:
                S.op("act", I("activation", out=out_ap, in_=pb[:, :], func=AF.Gelu_apprx_tanh), reads=Rpb, writes=Rout)
                yield
                return
            xs, tt_ = etmp[j], sg_sb[j]
            S.op("act", I("activation", out=xs[:], in_=pb[:, :], func=AF.Copy), reads=Rpb, writes=[Retmp[j]])
            yield
            S.op("dve", I("scalar_tensor_tensor", out=tt_[:], in0=xs[:], scalar=C1, in1=xs[:], op0=ALU.mult, op1=ALU.mult),
                 reads=[Retmp[j]], writes=[Rsg[j]])
            yield
            S.op("dve", I("scalar_tensor_tensor", out=tt_[:], in0=tt_[:], scalar=1.0, in1=xs[:], op0=ALU.add, op1=ALU.mult),
                 reads=[Retmp[j], Rsg[j]], writes=[Rsg[j]])
            yield
            S.op("act", I("activation", out=tt_[:], in_=tt_[:], func=AF.Exp, scale=-2.0 * C0), reads=[Rsg[j]], writes=[Rsg[j]])
            yield
            S.op("dve", I("tensor_scalar_add", out=tt_[:], in0=tt_[:], scalar1=1.0), reads=[Rsg[j]], writes=[Rsg[j]])
            yield
            S.op("dve", I("reciprocal", out=tt_[:], in_=tt_[:]), reads=[Rsg[j]], writes=[Rsg[j]])
            yield
            S.op("dve", I("tensor_tensor", out=out_ap, in0=xs[:], in1=tt_[:], op=ALU.mult), reads=[Retmp[j], Rsg[j]], writes=Rout)
            yield

        def inproj_lanes(l, blk, native=False):
            w_in_v = w_in_d[l].rearrange("(k p) c -> p k c", p=128)
            qTb, RqTb = qT2[blk % 2], RqT2[blk % 2]
            g3 = lambda ap: ap.rearrange("p (g d) -> p g d", d=64)

            def norm_lane(tts):
                yield from rstd_batch_gen([blk * 4 + tt for tt in tts])
                for tt in tts:
                    yield from norm_gen(blk * 4 + tt, hT, [RhT], tt * 128, 0)

            LB = {0: (banks[0], [Rbank[0]]), 1: (banks[1], [Rbank[1]]), 2: (bank2f, [Rbank[2]])}

            def fm_group(kind, sh, wsl, Rw, cc, j, lb):
                c = sh * 2 + cc
                pb, Rpb = LB[lb]
                S.op("pe", [I("matmul", pb[:, :], wsl[:, k, cc * 128:(cc + 1) * 128], hT[:, k, :],
                              start=(k == 0), stop=(k == 7)) for k in range(8)], reads=[Rw, RhT], writes=Rpb)
                yield
                if kind == "q":
                    S.op("act", I("activation", out=qTb[:, c, :], in_=pb[:, :], func=AF.Copy, scale=0.125),
                         reads=Rpb, writes=[RqTb])
                    yield
                elif kind == "k":
                    S.op("dve", I("tensor_copy", out=kT[:, c, blk * 512:(blk + 1) * 512], in_=pb[:, :]),
                         reads=Rpb, writes=[Rbig[c * 4 + blk]])
                    yield
                else:
                    yield from gelu_gen2(j, pb, Rpb, uT[:, c, :], [RuT], native)

            def tm_group(kind, w0, Rw0, w1, Rw1, tt, j, lb):
                t = blk * 4 + tt
                pb, Rpb = LB[lb]
                fns = []
                for hf, wsl in ((0, w0), (1, w1)):
                    fns += [I("matmul", pb[:, hf * 256:(hf + 1) * 256], hT[:, k, tt * 128:(tt + 1) * 128], wsl[:, k, :],
                              start=(k == 0), stop=(k == 7)) for k in range(8)]
                S.op("pe", fns, reads=[Rw0, Rw1, RhT], writes=Rpb)
                yield
                if kind == "v":
                    S.op("dve", I("tensor_copy", out=v_sb[:, t, :], in_=pb[:, :]), reads=Rpb, writes=[Rbig[16 + t]])
                    yield
                else:
                    yield from gelu_gen2(j, pb, Rpb, gate_n[:, tt, :], [Rgn[tt]], native)
                    S.op("pool", I("tensor_tensor", out=sqb[j][:], in0=gate_n[:, tt, :], in1=gate_n[:, tt, :], op=ALU.mult),
                         reads=[Rgn[tt]], writes=[Rsqb[j]])
                    yield
                    S.op("dve", I("tensor_reduce", out=gss[:, tt * 8:(tt + 1) * 8], in_=g3(sqb[j][:]), axis=AX.X, op=ALU.add),
                         reads=[Rsqb[j]], writes=[Rgst[tt]])
                    yield

            def proj_lane_a():
                for kind, sbase in (("k", 2), ("q", 0)):
                    for sh in range(2):
                        wsl, Rw, hd = ws.next(w_in_v[:, :, (sbase + sh) * 256:(sbase + sh + 1) * 256], [128, 8, 256])
                        for cc in range(2):
                            yield from fm_group(kind, sh, wsl, Rw, cc, 0, 2)
                        ws.release(hd)
                w0, Rw0, h0 = ws.next(w_in_v[:, :, 4 * 256:5 * 256], [128, 8, 256])
                w1, Rw1, h1 = ws.next(w_in_v[:, :, 5 * 256:6 * 256], [128, 8, 256])
                for tt in range(4):
                    yield from tm_group("v", w0, Rw0, w1, Rw1, tt, 0, 2)
                ws.release(h0)
                ws.release(h1)

            def proj_lane_b():
                slc = []
                hds = []
                for sh in range(2):
                    a_, b_, c_ = ws.next(w_in_v[:, :, (6 + sh) * 256:(7 + sh) * 256], [128, 8, 256])
                    slc.append((a_, b_))
                    hds.append(c_)
                w0, Rw0, h0 = ws.next(w_in_v[:, :, 8 * 256:9 * 256], [128, 8, 256])
                w1, Rw1, h1 = ws.next(w_in_v[:, :, 9 * 256:10 * 256], [128, 8, 256])
                hds += [h0, h1]
                gens = []
                for sh in range(2):
                    for cc in range(2):
                        gens.append(("u", sh, cc))
                for tt in range(4):
                    gens.append(("g", tt, 0))
                subs = [[], []]
                for idx, it in enumerate(gens):
                    subs[idx % 2].append(it)

                def sub(j):
                    for it in subs[j]:
                        if it[0] == "u":
                            wsl, Rw = slc[it[1]]
                            yield from fm_group("u", it[1], wsl, Rw, it[2], j, j)
                        else:
                            yield from tm_group("g", w0, Rw0, w1, Rw1, it[1], j, j)
                a, b = sub(0), sub(1)
                alive = [a, b]
                while alive:
                    nxt = []
                    for g in alive:
                        if next(g, "END") != "END":
                            nxt.append(g)
                    alive = nxt
                    yield
                for hd in hds:
                    ws.release(hd)

            def gate_fin_lane(tts):
                for tt in tts:
                    S.op("act", I("activation", out=glv[:, tt * 8:(tt + 1) * 8], in_=gss[:, tt * 8:(tt + 1) * 8], func=AF.Ln, bias=EPS,
                                  scale=1.0 / 64), reads=[Rgst[tt]], writes=[Rgst[tt]])
                    yield
                    S.op("act", I("activation", out=grs[:, tt * 8:(tt + 1) * 8], in_=glv[:, tt * 8:(tt + 1) * 8], func=AF.Exp, scale=-0.5),
                         reads=[Rgst[tt]], writes=[Rgst[tt]])
                    yield
                    S.op("dve", I("tensor_tensor", out=g3(gate_n[:, tt, :]), in0=g3(gate_n[:, tt, :]),
                                  in1=grs[:, tt * 8:(tt + 1) * 8].unsqueeze(2).broadcast_to([128, 8, 64]), op=ALU.mult),
                         reads=[Rgn[tt], Rgst[tt]], writes=[Rgn[tt]])
                    yield
                    S.op("dve", I("tensor_tensor", out=gate_n[:, tt, :], in0=gate_n[:, tt, :], in1=gsgu_bc[:], op=ALU.mult),
                         reads=[Rgn[tt], Rgsgu], writes=[Rgn[tt]])
                    yield

            return [[norm_lane((0, 1, 2, 3))], [proj_lane_a(), proj_lane_b()], [gate_fin_lane((0, 2)), gate_fin_lane((1, 3))]]

        def sgu_lanes(l, blk):
            def lane():
                for tt in range(4):
                    bk = acc_bank()
                    S.op("pe", [I("matmul", banks[bk][(g % 2) * 64:(g % 2) * 64 + 64, (g // 2) * 128:(g // 2 + 1) * 128],
                                  gate_n[:, tt, g * 64:(g + 1) * 64], WsT[:, l, g, :], start=True, stop=True, skip_group_check=True)
                                for g in range(8)], reads=[Rgn[tt], RWs], writes=[Rbank[bk]])
                    yield
                    S.op("dve", I("tensor_tensor", out=stmp[:], in0=banks[bk][:, :], in1=B_sgu[:].rearrange("p j t -> p (j t)"),
                                  op=ALU.add), reads=[Rbank[bk], RB], writes=[Rstmp])
                    yield
                    S.op("dve", I("tensor_tensor", out=y_sb[:, 4:8, tt * 128:(tt + 1) * 128],
                                  in0=stmp[:].rearrange("p (j t) -> p j t", t=128), in1=uT[:, :, tt * 128:(tt + 1) * 128], op=ALU.mult),
                         reads=[Rstmp, RuT], writes=Ry[4:8])
                    yield
            return [[lane()]]

        def ynorm_outproj_lanes(l, blk):
            w_out_v = w_out_d[l].rearrange("(k p) c -> p k c", p=128)

            def ynorm_lane(i):
                for c in range(i, 8, 2):
                    S.op("pool", I("tensor_tensor", out=sqb[i][:], in0=y_sb[:, c, :], in1=y_sb[:, c, :], op=ALU.mult),
                         reads=[Ry[c]], writes=[Rsqb[i]])
                    yield
                    bk = i
                    S.op("pe", I("matmul", banks[bk][:, :], bones[:], sqb[i][:], start=True, stop=True),
                         reads=[Rsqb[i], Rconst], writes=[Rbank[bk]])
                    yield
                    S.op("act", I("activation", out=nl[i][:], in_=banks[bk][:, :], func=AF.Ln, bias=EPS, scale=1.0 / 64),
                         reads=[Rbank[bk]], writes=[Rnl[i]])
                    yield
                    S.op("act", I("activation", out=nl[i][:], in_=nl[i][:], func=AF.Exp, scale=-0.5), reads=[Rnl[i]], writes=[Rnl[i]])
                    yield
                    S.op("dve", I("scalar_tensor_tensor", out=y_sb[:, c, :], in0=y_sb[:, c, :], scalar=gout_c[:, c:c + 1],
                                  in1=nl[i][:], op0=ALU.mult, op1=ALU.mult), reads=[Ry[c], Rgout, Rnl[i]], writes=[Ry[c]])
                    yield

            def outproj_lane():
                for hf in range(2):
                    w0, Rw0, h0 = ws.next(w_out_v[:, :, (2 * hf) * 256:(2 * hf + 1) * 256], [128, 8, 256])
                    w1, Rw1, h1 = ws.next(w_out_v[:, :, (2 * hf + 1) * 256:(2 * hf + 2) * 256], [128, 8, 256])
                    for tt in range(4):
                        t = blk * 4 + tt
                        bk = acc_bank()
                        fns = []
                        for q_, wsl in ((0, w0), (1, w1)):
                            fns += [I("matmul", banks[bk][:, q_ * 256:(q_ + 1) * 256], y_sb[:, k, tt * 128:(tt + 1) * 128], wsl[:, k, :],
                                      start=(k == 0), stop=(k == 7)) for k in range(8)]
                        S.op("pe", fns, reads=[Rw0, Rw1] + Ry, writes=[Rbank[bk]])
                        yield
                        xs = x_sb[:, t, hf * 512:(hf + 1) * 512]
                        S.op("dve", I("tensor_tensor", out=xs, in0=xs, in1=banks[bk][:, :], op=ALU.add),
                             reads=[Rbank[bk], Rx[t]], writes=[Rx[t]])
                        yield
                    ws.release(h0)
                    ws.release(h1)
            return [[ynorm_lane(0), ynorm_lane(1)], [outproj_lane()]]

        def chain(*gens):
            for g in gens:
                yield from g

        def run(g):
            for _ in g:
                pass

        def phaseA(l, pre=None):
            load_gain(g_mix_d[l])
            S.dma("sp", I("dma_start", out=gsgu_bc[:], in_=g_sgu_d[l].partition_broadcast(128)), writes=[Rgsgu])
            for hh in range(2):
                S.dma("sp", I("dma_start", out=B_sgu[hh * 64:(hh + 1) * 64, :, :],
                              in_=sgu_b_d[l].rearrange("(j two) t -> two j t", two=2)[hh].partition_broadcast(64)), writes=[RB])
            S.dma("sp", I("dma_start", out=gout_c[:], in_=g_out_d[l].rearrange("(c p) -> p c", p=128),
                          allow_slow_non_contiguous=True), writes=[Rgout])
            if pre:
                for f in pre:
                    f()
            Lanes(inproj_lanes(l, 0, True) + sgu_lanes(l, 0), 1).tick(10 ** 6)
            for blk in range(4):
                first = None
                stages = []
                if blk > 0:
                    first = Lanes(ynorm_outproj_lanes(l, blk - 1), 36)
                    stages += sgu_lanes(l, blk)
                if blk < 3:
                    stages += inproj_lanes(l, blk + 1, blk == 0)
                side = Lanes(stages, (12 if blk > 0 else 0) + (85 if blk < 3 else 0)) if stages else None
                attention_block(blk, side, first)
            Lanes(ynorm_outproj_lanes(l, 3), 1).tick(10 ** 6)

        pending = []
        dacc = [0]
        DBANKS = (0, 1, 7)

        def ffn_core(wg_v, wu_v, wd_rows, nch, gate_col):
            wgs, Rwg, hg = ws.next(wg_v, [128, 8, nch * 128])
            wus, Rwu, hu = ws.next(wu_v, [128, 8, nch * 128])
            wds, Rwd, hdn = ws.next(wd_rows.rearrange("(fc p) d -> p fc d", p=128), [128, nch, D])

            def gu_parts(tb, ai):
                hsegs = [Rbig[k * 4 + tb] for k in range(8)]
                parts = []
                for fc in range(nch):
                    st_ = {}

                    def g_part(fc=fc, st_=st_):
                        bg = 3 + (cnt["z"] % 2)
                        cnt["z"] += 1
                        st_["bg"] = bg
                        S.op("pe", [I("matmul", banks[bg][:, :], wgs[:, k, fc * 128:(fc + 1) * 128], h2T[:, k, tb * 512:(tb + 1) * 512],
                                      start=(k == 0), stop=(k == 7)) for k in range(8)], reads=[Rwg] + hsegs, writes=[Rbank[bg]])
                        S.op("act", I("activation", out=sg_sb[fc % 2][:], in_=banks[bg][:, :], func=AF.Silu),
                             reads=[Rbank[bg]], writes=[Rsg[fc % 2]])

                    def u_part(fc=fc, st_=st_):
                        bu = 5 + (cnt["c"] % 2)
                        cnt["c"] += 1
                        S.op("pe", [I("matmul", banks[bu][:, :], wus[:, k, fc * 128:(fc + 1) * 128], h2T[:, k, tb * 512:(tb + 1) * 512],
                                      start=(k == 0), stop=(k == 7)) for k in range(8)], reads=[Rwu] + hsegs, writes=[Rbank[bu]])
                        S.op("dve", I("tensor_tensor", out=aT[ai][:, fc, :], in0=sg_sb[fc % 2][:], in1=banks[bu][:, :], op=ALU.mult),
                             reads=[Rsg[fc % 2], Rbank[bu]], writes=[RaT[ai]])
                    parts += [g_part, u_part]
                return parts

            def down_parts(tb, ai):
                parts = []
                for tt in range(4):
                    for hf in range(2):
                        def d_part(tt=tt, hf=hf):
                            t = tb * 4 + tt
                            bk = DBANKS[dacc[0] % 3]
                            dacc[0] += 1
                            S.op("pe", [I("matmul", banks[bk][:, :], aT[ai][:, fc, tt * 128:(tt + 1) * 128], wds[:, fc, hf * 512:(hf + 1) * 512],
                                          start=(fc == 0), stop=(fc == nch - 1)) for fc in range(nch)],
                                 reads=[Rwd, RaT[ai]], writes=[Rbank[bk]])
                            xs = x_sb[:, t, hf * 512:(hf + 1) * 512]
                            if gate_col is None:
                                S.op("dve", I("tensor_tensor", out=xs, in0=xs, in1=banks[bk][:, :], op=ALU.add),
                                     reads=[Rbank[bk], Rx[t]], writes=[Rx[t]])
                            else:
                                S.op("dve", I("scalar_tensor_tensor", out=xs, in0=banks[bk][:, :], scalar=gates[:, t, gate_col:gate_col + 1],
                                              in1=xs, op0=ALU.mult, op1=ALU.add), reads=[Rbank[bk], Rx[t], Rgates[t]], writes=[Rx[t]])
                        parts.append(d_part)
                return parts

            for tb in range(4):
                ai = cnt["u"] % 2
                cnt["u"] += 1
                gp = gu_parts(tb, ai)
                dp = list(pending)
                del pending[:]
                per = (len(dp) + len(gp) - 1) // len(gp)
                for g in gp:
                    g()
                    for _ in range(per):
                        if dp:
                            dp.pop(0)()
                while dp:
                    dp.pop(0)()
                pending.extend(down_parts(tb, ai))
            ws.release(hg)
            ws.release(hu)
            pending.append(lambda: ws.release(hdn))

        def ffn_flush():
            while pending:
                pending.pop(0)()

        def router_tile(t):
            bk = acc_bank()
            S.op("pe", [I("matmul", banks[bk][:, 0:NE], h2T[:, k, t * 128:(t + 1) * 128], router_sb[:, k, :],
                          start=(k == 0), stop=(k == 7)) for k in range(8)],
                 reads=[Rrouter] + [Rbig[k * 4 + t // 4] for k in range(8)], writes=[Rbank[bk]])
            L = dict(reads=[Rlog], writes=[Rlog])
            S.op("dve", I("tensor_copy", out=logit[:], in_=banks[bk][:, 0:NE]), reads=[Rbank[bk]], writes=[Rlog])
            S.op("dve", I("tensor_reduce", out=m1[:, 0:1], in_=logit[:], axis=AX.X, op=ALU.max), **L)
            S.op("dve", I("tensor_scalar", out=mk1[:], in0=logit[:], scalar1=m1[:, 0:1], scalar2=None, op0=ALU.is_equal), **L)
            S.op("dve", I("scalar_tensor_tensor", out=l2[:], in0=mk1[:], scalar=-1e30, in1=logit[:], op0=ALU.mult, op1=ALU.add), **L)
            S.op("dve", I("tensor_reduce", out=m1[:, 1:2], in_=l2[:], axis=AX.X, op=ALU.max), **L)
            S.op("dve", I("tensor_scalar", out=mk2[:], in0=l2[:], scalar1=m1[:, 1:2], scalar2=None, op0=ALU.is_equal), **L)
            S.op("dve", I("tensor_tensor", out=m1[:, 2:3], in0=m1[:, 1:2], in1=m1[:, 0:1], op=ALU.subtract), **L)
            S.op("act", I("activation", out=m1[:, 2:3], in_=m1[:, 2:3], func=AF.Exp), **L)
            S.op("dve", I("tensor_scalar_add", out=m1[:, 3:4], in0=m1[:, 2:3], scalar1=1.0), **L)
            S.op("dve", I("reciprocal", out=m1[:, 3:4], in_=m1[:, 3:4]), **L)
            S.op("dve", I("tensor_tensor", out=m1[:, 2:3], in0=m1[:, 2:3], in1=m1[:, 3:4], op=ALU.mult), **L)
            S.op("dve", I("tensor_scalar", out=mk1[:], in0=mk1[:], scalar1=m1[:, 3:4], scalar2=None, op0=ALU.mult), **L)
            S.op("dve", I("scalar_tensor_tensor", out=gates[:, t, :], in0=mk2[:], scalar=m1[:, 2:3], in1=mk1[:],
                          op0=ALU.mult, op1=ALU.add), reads=[Rlog], writes=[Rgates[t]])

        def phaseB(l):
            load_gain(g_ffn_d[l])
            for q4 in range(4):
                run(rstd_batch_gen(list(range(q4 * 4, q4 * 4 + 4))))
            for t in range(NT):
                norm_tile(t, h2T, [Rbig[k * 4 + t // 4] for k in range(8)], t * 128)
            i = l // 2
            if l % 2 == 0:
                wg_v = ffn_wg_d[i].rearrange("(k p) c -> p k c", p=128)
                wu_v = ffn_wu_d[i].rearrange("(k p) c -> p k c", p=128)
                for fg in range(D_FF // 256):
                    ffn_core(wg_v[:, :, fg * 256:(fg + 1) * 256], wu_v[:, :, fg * 256:(fg + 1) * 256],
                             ffn_wd_d[i][fg * 256:(fg + 1) * 256, :], 2, None)
                ffn_flush()
            else:
                for t in range(NT):
                    router_tile(t)
                for ex in range(NE):
                    wg_v = moe_wg_d[i, ex].rearrange("(k p) c -> p k c", p=128)
                    wu_v = moe_wu_d[i, ex].rearrange("(k p) c -> p k c", p=128)
                    f0 = 0
                    while f0 < D_FFE:
                        nch = min(2, (D_FFE - f0) // 128)
                        ffn_core(wg_v[:, :, f0:f0 + nch * 128], wu_v[:, :, f0:f0 + nch * 128],
                                 moe_wd_d[i, ex][f0:f0 + nch * 128, :], nch, ex)
                        f0 += nch * 128
                ffn_flush()

        fin = []

        def program():
            setup_consts()
            for s in range(nseq):
                for t in range(4):
                    S.dma("sp", I("dma_start", out=x_sb[:, t, :], in_=x_d[s, t * 128:(t + 1) * 128, :]), writes=[Rx[t]])
                rest = [(lambda t=t, s=s: S.dma("sp", I("dma_start", out=x_sb[:, t, :], in_=x_d[s, t * 128:(t + 1) * 128, :]),
                                               writes=[Rx[t]])) for t in range(4, NT)]
                done = False
                for l in range(depth):
                    phaseA(l, rest if l == 0 else None)
                    if stop == "A%d" % l:
                        done = True
                        break
                    phaseB(l)
                    if stop == "F%d" % l:
                        done = True
                        break
                if not done:
                    load_gain(g_final_d)
                    for q4 in range(4):
                        run(rstd_batch_gen(list(range(q4 * 4, q4 * 4 + 4))))
                    for t in range(NT):
                        S.op("dve", I("scalar_tensor_tensor", out=x_sb[:, t, :], in0=x_sb[:, t, :], scalar=rstd[:, t:t + 1],
                                      in1=gain_bc[:], op0=ALU.mult, op1=ALU.mult), reads=[Rx[t], Rss[t], Rgain], writes=[Rx[t]])
                for t in range(NT):
                    stp = S.dma("sp", I("dma_start", out=out_d[s, t * 128:(t + 1) * 128, :], in_=x_sb[:, t, :]), reads=[Rx[t]])
                    if stp is not None:
                        fin.append(stp)

        S.dry = True
        ws.record = True
        program()
        S.dry = False
        ws.start_real()
        for k in cnt:
            cnt[k] = 0
        acc_rr[0] = 0
        dacc[0] = 0
        program()
        S.final_wait("sp", fin)
        print("sbuf remaining", nc.sbuf_bytes_remaining, "ops", S.n_ins, "waits", S.n_wait, "wslices", len(ws.plan))
        with nc.Block() as block:
            S.emit(block)
    return nc


_WEIGHT_NAMES = ["w_in", "w_out", "g_mix", "g_ffn", "g_sgu", "sgu_w", "sgu_b", "g_out", "ffn_w_gate", "ffn_w_up",
                 "ffn_w_down", "router_w", "moe_w_gate", "moe_w_up", "moe_w_down", "g_final"]


def kernel(**inputs):
    x = np.ascontiguousarray(np.asarray(inputs["x"], dtype=np.float32))
    wts = {n: np.ascontiguousarray(np.asarray(inputs[n], dtype=np.float32)) for n in _WEIGHT_NAMES}
    nc = build()
    in_maps = []
    for c in range(NCORES):
        m = {"x": x[c * SEQ_PER_CORE:(c + 1) * SEQ_PER_CORE]}
        m.update(wts)
        in_maps.append(m)
    res = run_bass_kernel_spmd(nc, in_maps, core_ids=list(range(NCORES)))
    out = np.concatenate([np.asarray(r["out"]) for r in res.results], axis=0)
    return out.astype(np.float32, copy=False)
```
